# Optimizing a Trainium2 kernel written in Bass

```python
import jax, jax.numpy as jnp
from jax import lax
import numpy as np

D_MODEL = 1024
BATCH = 4
SEQ = 8192
DEPTH = 4

CHUNK = 64
N_MIXERS = 2
N_LAYERS_A = (DEPTH + N_MIXERS - 1) // N_MIXERS
N_LAYERS_B = DEPTH // N_MIXERS
CONV_A_WIDTH = 31
CONV_B_WIDTH = 3
N_GROUPS = 4
EXPERTS_PER_GROUP = 8
N_EXPERTS = N_GROUPS * EXPERTS_PER_GROUP
TOP_K = 2
D_EXPERT = D_MODEL // 2
ROUTE_BLOCK = 128
ALPHA = (2.0 * DEPTH) ** 0.25
BETA = (8.0 * DEPTH) ** -0.25
LN_EPS = 1e-5

kernel_name = "hybrid_conv_hmoe_deepnorm_adaln"


def layer_norm(x, g, b):
    xf = x.astype(jnp.float32)
    mu = jnp.mean(xf, axis=-1, keepdims=True)
    var = jnp.mean(jnp.square(xf - mu), axis=-1, keepdims=True)
    y = (xf - mu) * lax.rsqrt(var + LN_EPS)
    return (y * g.astype(jnp.float32) + b.astype(jnp.float32)).astype(x.dtype)


def causal_depthwise_conv(u, w):
    k = w.shape[0]
    return lax.conv_general_dilated(
        u, w[:, None, :].astype(u.dtype), window_strides=(1,), padding=[(k - 1, 0)],
        dimension_numbers=("NWC", "WIO", "NWC"), feature_group_count=u.shape[-1])


def conformer_conv(h, w_in, b_in, w_dw, b_dw, ln_g, ln_b, w_out, b_out):
    a, g = jnp.split(h @ w_in + b_in, 2, axis=-1)
    u = a * jax.nn.sigmoid(g)
    u = causal_depthwise_conv(u, w_dw) + b_dw
    u = jax.nn.silu(layer_norm(u, ln_g, ln_b))
    return u @ w_out + b_out


def short_gated_conv(h, w_in, w_dw, w_out):
    gb, gc, v = jnp.split(h @ w_in, 3, axis=-1)
    u = causal_depthwise_conv(gc * v, w_dw)
    return (gb * u) @ w_out


def hierarchical_moe(h, w_group, b_group, w_router, b_router, w_gate, w_up, w_down):
    bsz, seq, d = h.shape
    t = bsz * seq
    xf = h.reshape(t, d)
    g_logits = (xf @ w_group + b_group).astype(jnp.float32)
    g_probs = jax.nn.softmax(g_logits, axis=-1)
    g_idx = jnp.argmax(g_logits, axis=-1).astype(jnp.int32)
    g_w = jnp.take_along_axis(g_probs, g_idx[:, None], axis=-1)
    e_logits = (xf @ w_router + b_router).astype(jnp.float32).reshape(t, N_GROUPS, EXPERTS_PER_GROUP)
    e_logits = jnp.take_along_axis(e_logits, g_idx[:, None, None], axis=1)[:, 0]
    e_probs = jax.nn.softmax(e_logits, axis=-1)
    top_p, top_e = lax.top_k(e_probs, TOP_K)
    top_p = top_p / jnp.sum(top_p, axis=-1, keepdims=True)
    weights = (g_w * top_p).reshape(-1)
    flat_e = (g_idx[:, None] * EXPERTS_PER_GROUP + top_e.astype(jnp.int32)).reshape(-1)
    n_assign = t * TOP_K
    order = jnp.argsort(flat_e)
    sorted_e = flat_e[order]
    sorted_tok = (order // TOP_K).astype(jnp.int32)
    counts = jnp.zeros((N_EXPERTS,), jnp.int32).at[flat_e].add(1)
    starts = jnp.cumsum(counts) - counts
    padded = (counts + ROUTE_BLOCK - 1) // ROUTE_BLOCK * ROUTE_BLOCK
    pad_ends = jnp.cumsum(padded)
    pad_starts = pad_ends - padded
    dest = pad_starts[sorted_e] + (jnp.arange(n_assign, dtype=jnp.int32) - starts[sorted_e])
    n_slots = n_assign + N_EXPERTS * ROUTE_BLOCK
    n_blocks = n_slots // ROUTE_BLOCK
    slot_tok = jnp.full((n_slots,), t, jnp.int32).at[dest].set(sorted_tok)
    block_start = jnp.arange(n_blocks, dtype=jnp.int32) * ROUTE_BLOCK
    block_e = jnp.minimum(jnp.sum(pad_ends[None, :] <= block_start[:, None], axis=-1), N_EXPERTS - 1)
    x_pad = jnp.concatenate([xf, jnp.zeros((1, d), xf.dtype)], axis=0)
    xb = x_pad[slot_tok].reshape(n_blocks, ROUTE_BLOCK, d)

    def expert_block(args):
        xblk, e = args
        hid = jax.nn.silu(xblk @ w_gate[e]) * (xblk @ w_up[e])
        return hid @ w_down[e]

    yb = lax.map(expert_block, (xb, block_e)).reshape(n_slots, d)
    contrib = weights[order][:, None].astype(xf.dtype) * yb[dest]
    y = jnp.zeros((t, d), xf.dtype).at[sorted_tok].add(contrib)
    return y.reshape(bsz, seq, d)


def setup_inputs(seed: int = 0) -> dict:
    key = jax.random.key(seed)
    ks = jax.random.split(key, 26)
    D, F = D_MODEL, D_EXPERT
    nrm = lambda k, shape, s: jax.random.normal(k, shape, jnp.float32) * s
    return {
        "x": nrm(ks[0], (BATCH, SEQ, D), 1.0),
        "c": nrm(ks[1], (BATCH, D), 1.0),
        "ada_w": nrm(ks[2], (DEPTH, D, 6 * D), 0.5 * D ** -0.5),
        "ada_b": nrm(ks[3], (DEPTH, 6 * D), 0.01),
        "a_w_in": nrm(ks[4], (N_LAYERS_A, D, 2 * D), D ** -0.5),
        "a_b_in": nrm(ks[5], (N_LAYERS_A, 2 * D), 0.01),
        "a_w_dw": nrm(ks[6], (N_LAYERS_A, CONV_A_WIDTH, D), CONV_A_WIDTH ** -0.5),
        "a_b_dw": nrm(ks[7], (N_LAYERS_A, D), 0.01),
        "a_ln_g": 1.0 + nrm(ks[8], (N_LAYERS_A, D), 0.01),
        "a_ln_b": nrm(ks[9], (N_LAYERS_A, D), 0.01),
        "a_w_out": nrm(ks[10], (N_LAYERS_A, D, D), BETA * D ** -0.5),
        "a_b_out": nrm(ks[11], (N_LAYERS_A, D), 0.01),
        "b_w_in": nrm(ks[12], (N_LAYERS_B, D, 3 * D), D ** -0.5),
        "b_w_dw": nrm(ks[13], (N_LAYERS_B, CONV_B_WIDTH, D), CONV_B_WIDTH ** -0.5),
        "b_w_out": nrm(ks[14], (N_LAYERS_B, D, D), BETA * D ** -0.5),
        "mix_ln_g": 1.0 + nrm(ks[15], (DEPTH, D), 0.01),
        "mix_ln_b": nrm(ks[16], (DEPTH, D), 0.01),
        "ffn_ln_g": 1.0 + nrm(ks[17], (DEPTH, D), 0.01),
        "ffn_ln_b": nrm(ks[18], (DEPTH, D), 0.01),
        "r_w_group": nrm(ks[19], (DEPTH, D, N_GROUPS), D ** -0.5),
        "r_b_group": nrm(ks[20], (DEPTH, N_GROUPS), 0.01),
        "r_w_expert": nrm(ks[21], (DEPTH, D, N_EXPERTS), D ** -0.5),
        "r_b_expert": nrm(ks[22], (DEPTH, N_EXPERTS), 0.01),
        "e_w_gate": nrm(ks[23], (DEPTH, N_EXPERTS, D, F), D ** -0.5),
        "e_w_up": nrm(ks[24], (DEPTH, N_EXPERTS, D, F), D ** -0.5),
        "e_w_down": nrm(ks[25], (DEPTH, N_EXPERTS, F, D), BETA * F ** -0.5),
    }


def reference(x, c, ada_w, ada_b, a_w_in, a_b_in, a_w_dw, a_b_dw, a_ln_g, a_ln_b, a_w_out, a_b_out,
              b_w_in, b_w_dw, b_w_out, mix_ln_g, mix_ln_b, ffn_ln_g, ffn_ln_b,
              r_w_group, r_b_group, r_w_expert, r_b_expert, e_w_gate, e_w_up, e_w_down):
    c_act = jax.nn.silu(c)
    for i in range(DEPTH):
        mods = (c_act @ ada_w[i] + ada_b[i])[:, None, :]
        sh_m, sc_m, g_m, sh_f, sc_f, g_f = jnp.split(mods, 6, axis=-1)
        h = x * (1.0 + sc_m) + sh_m
        j = i // N_MIXERS
        if i % N_MIXERS == 0:
            y = conformer_conv(h, a_w_in[j], a_b_in[j], a_w_dw[j], a_b_dw[j],
                               a_ln_g[j], a_ln_b[j], a_w_out[j], a_b_out[j])
        else:
            y = short_gated_conv(h, b_w_in[j], b_w_dw[j], b_w_out[j])
        x = layer_norm(ALPHA * x + (1.0 + g_m) * y, mix_ln_g[i], mix_ln_b[i])
        h = x * (1.0 + sc_f) + sh_f
        y = hierarchical_moe(h, r_w_group[i], r_b_group[i], r_w_expert[i], r_b_expert[i],
                             e_w_gate[i], e_w_up[i], e_w_down[i])
        x = layer_norm(ALPHA * x + (1.0 + g_f) * y, ffn_ln_g[i], ffn_ln_b[i])
    return x
```

```python
import numpy as np
from contextlib import ExitStack
import concourse.bass as bass
import concourse.mybir as mybir
from concourse.bass_utils import run_bass_kernel_spmd

F32 = mybir.dt.float32
BF16 = mybir.dt.bfloat16
I32 = mybir.dt.int32
ALU = mybir.AluOpType
AF = mybir.ActivationFunctionType
AX = mybir.AxisListType

NCORES = 8
D = 1024
KC = 8
HALO = 128
NOWN = 4096
NTOK = NOWN + HALO
NTILE = NTOK // 128
NEXP = 32
DE = 512
BS = 384
NSUB = BS // 128
NBIG = (2 * NTOK) // BS + NEXP
NSLOT = NBIG * BS
DEPTH = 4
ALPHA = (2.0 * DEPTH) ** 0.25
EPS = 1e-5
WA = 256
ENGS = ("pe", "act", "dve", "pool", "sp")

_names = {}


def sem_name(sem):
    k = id(sem)
    if k not in _names:
        _names[k] = "sem%d" % len(_names)
    return _names[k]


class Tracker:
    def __init__(self, nc, es, n_dma_sems=24):
        self.nc = nc
        self.eng = {"pe": nc.tensor, "act": nc.scalar, "dve": nc.vector, "pool": nc.gpsimd, "sp": nc.sync}
        self.psem = {e: es.enter_context(nc.semaphore("prog_" + e)) for e in ("pe", "act", "dve", "pool")}
        self.pcount = {e: 0 for e in self.psem}
        self.dsem = {}
        for q in ("sp", "pool"):
            self.dsem[q] = [[es.enter_context(nc.semaphore(f"dma_{q}{i}")), 0] for i in range(n_dma_sems)]
        self.drr = {q: 0 for q in self.dsem}
        self.waited = {}
        self.writers = {}
        self.readers = {}

    def _wait(self, e, ev):
        sem, name, val = ev
        k = (e, name)
        if self.waited.get(k, 0) >= val:
            return
        self.waited[k] = val
        self.eng[e].wait_ge(sem, val)

    def _deps(self, reads, writes, swrites):
        deps = []
        for k in reads:
            deps += self.writers.get(k, [])
        for k in writes:
            deps += self.writers.get(k, []) + self.readers.get(k, [])
        for k in swrites:
            deps += self.readers.get(k, [])
        return deps

    def _record(self, ev, reads, writes, swrites):
        for k in reads:
            self.readers.setdefault(k, []).append(ev)
        for k in writes:
            self.writers[k] = [ev]
            self.readers[k] = []
        for k in swrites:
            self.writers.setdefault(k, []).append(ev)

    def op(self, e, fn, reads=(), writes=(), swrites=(), signal=True, extra_deps=()):
        skip_same = (e == "pe")
        for ev in list(self._deps(reads, writes, swrites)) + list(extra_deps):
            if skip_same and ev[1] == "prog_pe":
                continue
            self._wait(e, ev)
        ins = fn()
        if signal:
            self.pcount[e] += 1
            ins.then_inc(self.psem[e], 1)
        ev = (self.psem[e], "prog_" + e, self.pcount[e] if signal else self.pcount[e] + 1)
        self._record(ev, reads, writes, swrites)
        return ev

    def dma(self, q, fn, reads=(), writes=(), swrites=(), extra_deps=()):
        for ev in list(self._deps(reads, writes, swrites)) + list(extra_deps):
            self._wait(q, ev)
        slot = self.dsem[q][self.drr[q] % len(self.dsem[q])]
        self.drr[q] += 1
        sem, cnt = slot
        name = sem_name(sem)
        if cnt > 0:
            self._wait(q, (sem, name, cnt))
        ins = fn()
        slot[1] = cnt + 16
        ins.then_inc(sem, 16)
        ev = (sem, name, cnt + 16)
        self._record(ev, reads, writes, swrites)
        return ev

    def all_events(self):
        evs = []
        for e, s in self.psem.items():
            if self.pcount[e] > 0:
                evs.append((s, "prog_" + e, self.pcount[e]))
        for q, lst in self.dsem.items():
            for sem, cnt in lst:
                if cnt > 0:
                    evs.append((sem, sem_name(sem), cnt))
        return evs

    def barrier(self, engines=ENGS):
        evs = self.all_events()
        for e in engines:
            for ev in evs:
                self._wait(e, ev)
        self.writers.clear()
        self.readers.clear()


class Prog:
    def __init__(self, layers, first, last, debug=(), stop=None):
        self.stop = stop
        self.layers = list(layers)
        self.first = first
        self.last = last
        self.debug = debug
        self.pending = []
        self.nc = bass.Bass("TRN2", target_bir_lowering=False)
        self.es = ExitStack()
        self.build()

    def pump(self, steps=2, drain=False, upto=None):
        n = 0
        while self.pending:
            tile, g = self.pending[0]
            if not drain and not (upto is not None and tile <= upto) and n >= steps:
                break
            try:
                next(g)
                n += 1
            except StopIteration:
                self.pending.pop(0)

    def din(self, name, shape, dt=F32):
        return self.nc.dram_tensor(name, list(shape), dt, kind="ExternalInput").ap()

    def dscr(self, name, shape, dt=F32):
        return self.nc.dram_tensor(name, list(shape), dt, kind="Internal").ap()

    def sb(self, es, name, shape, dt):
        self._uid = getattr(self, "_uid", 0) + 1
        return es.enter_context(self.nc.sbuf_tensor("s%d_%s" % (self._uid, name), list(shape), dt))

    def ps(self, es, name, shape, dt=F32):
        self._uid = getattr(self, "_uid", 0) + 1
        return es.enter_context(self.nc.psum_tensor("p%d_%s" % (self._uid, name), list(shape), dt))

    def build(self):
        nc, es = self.nc, self.es
        T = self.T = Tracker(nc, es)
        pe, act, dve, pool, sp = nc.tensor, nc.scalar, nc.vector, nc.gpsimd, nc.sync
        I = self.I = {}
        I["x_in"] = self.din("x_in", [NTOK, D])
        I["flag"] = self.din("flag", [128, 1])
        I["c_col"] = self.din("c_col", [128, KC])
        I["ident_f"] = self.din("ident_f", [128, 128])
        I["tri_f"] = self.din("tri_f", [128, 128])
        I["blk_start"] = self.din("blk_start", [128, NBIG])
        I["iota_p"] = self.din("iota_p", [128, 1])
        I["ada_w"] = self.din("ada_w", [DEPTH, D, 6 * D])
        I["ada_b"] = self.din("ada_b", [DEPTH, 6 * D])
        I["a_w_in"] = self.din("a_w_in", [2, D, 2 * D])
        I["a_b_in_c"] = self.din("a_b_in_c", [2, 128, 16])
        I["a_w_dw_c"] = self.din("a_w_dw_c", [2, 128, KC, 31])
        I["a_b_dw_c"] = self.din("a_b_dw_c", [2, 128, KC])
        I["a_ln_g_c"] = self.din("a_ln_g_c", [2, 128, KC])
        I["a_ln_b_c"] = self.din("a_ln_b_c", [2, 128, KC])
        I["a_w_out"] = self.din("a_w_out", [2, D, D])
        I["a_b_out"] = self.din("a_b_out", [2, D])
        I["b_w_in"] = self.din("b_w_in", [2, D, 3 * D])
        I["b_w_dw_c"] = self.din("b_w_dw_c", [2, 128, KC, 3])
        I["b_w_out"] = self.din("b_w_out", [2, D, D])
        I["mix_ln_g"] = self.din("mix_ln_g", [DEPTH, D])
        I["mix_ln_b"] = self.din("mix_ln_b", [DEPTH, D])
        I["ffn_ln_g"] = self.din("ffn_ln_g", [DEPTH, D])
        I["ffn_ln_b"] = self.din("ffn_ln_b", [DEPTH, D])
        I["mix_ln_g_c"] = self.din("mix_ln_g_c", [DEPTH, 128, KC])
        I["mix_ln_b_c"] = self.din("mix_ln_b_c", [DEPTH, 128, KC])
        I["r_w"] = self.din("r_w", [DEPTH, D, 36])
        I["r_b"] = self.din("r_b", [DEPTH, 36])
        if self.stop not in ("mods", "mixer", "route", "scatter"):
            for nm in ("e_w_gate", "e_w_up", "e_w_down"):
                for l_ in self.layers:
                    for hh in range(2):
                        key = "%s_%d_%d" % (nm, l_, hh)
                        I[key] = self.din(key, [NEXP * 128, 2048])
        self.out = nc.dram_tensor("out", [NTOK if not self.last else NOWN, D], F32, kind="ExternalOutput").ap()
        self.dbg = {}
        for name, shape, dt in self.debug:
            self.dbg[name] = nc.dram_tensor(name, list(shape), dt, kind="ExternalOutput").ap()

        S = self.S = {}
        S["xres"] = self.dscr("xres", [NTOK, D])
        S["xnb"] = self.dscr("xnb", [NTOK, D], BF16)
        S["xs"] = self.dscr("xs", [NSLOT, D], BF16)
        S["yb"] = self.dscr("yb", [NSLOT, D])
        S["mods"] = self.dscr("mods", [DEPTH, 6 * D])

        C = self.C = {}
        C["ident_f"] = self.sb(es, "ident_f", [128, 128], F32)
        C["ident_b"] = self.sb(es, "ident_b", [128, 128], BF16)
        C["ones_b"] = self.sb(es, "ones_b", [128, 128], BF16)
        C["ones_f"] = self.sb(es, "ones_f", [128, 128], F32)
        C["tri_f"] = self.sb(es, "tri_f", [128, 128], F32)
        C["blk_start"] = self.sb(es, "blk_start", [128, NBIG], F32)
        C["iota_p"] = self.sb(es, "iota_p", [128, 1], F32)
        C["flag"] = self.sb(es, "flag", [128, 1], F32)
        C["eps"] = self.sb(es, "eps", [128, 1], F32)
        C["c_col"] = self.sb(es, "c_col", [128, KC], F32)
        C["c_act"] = self.sb(es, "c_act", [128, KC], BF16)
        C["dummy"] = self.sb(es, "dummy", [128, 1], F32)
        C["kc"] = self.sb(es, "kc", [128, 4], F32)
        R = self.R = {}
        R["oh1"] = self.sb(es, "oh1", [128, NTILE, NEXP], BF16)
        R["oh2"] = self.sb(es, "oh2", [128, NTILE, NEXP], BF16)
        R["pos1"] = self.sb(es, "pos1", [128, NTILE], F32)
        R["pos2"] = self.sb(es, "pos2", [128, NTILE], F32)
        R["w1"] = self.sb(es, "w1", [128, NTILE], F32)
        R["w2"] = self.sb(es, "w2", [128, NTILE], F32)
        R["dest1"] = self.sb(es, "dest1", [128, NTILE], I32)
        R["dest2"] = self.sb(es, "dest2", [128, NTILE], I32)
        R["tot"] = self.sb(es, "tot", [128, NEXP], F32)
        R["widx"] = self.sb(es, "widx", [128, NBIG], I32)
        L = self.L = {}
        L["gB"] = self.sb(es, "gB", [128, D], F32)
        L["lngB"] = self.sb(es, "lngB", [128, D], F32)
        L["lnbB"] = self.sb(es, "lnbB", [128, D], F32)
        L["modc"] = self.sb(es, "modc", [128, 6, KC], F32)
        L["sc1m"] = self.sb(es, "sc1m", [128, KC], F32)
        L["A2c"] = self.sb(es, "A2c", [128, KC], F32)
        L["B2c"] = self.sb(es, "B2c", [128, KC], F32)
        L["lngc"] = self.sb(es, "lngc", [128, KC], F32)
        L["lnbc"] = self.sb(es, "lnbc", [128, KC], F32)
        L["rw"] = self.sb(es, "rw", [128, KC, 36], F32)
        L["rwf"] = self.sb(es, "rwf", [128, KC, 36], F32)
        L["rbrow"] = self.sb(es, "rbrow", [1, 36], F32)
        L["b_in_c"] = self.sb(es, "b_in_c", [128, 16], F32)
        L["wdw_c"] = self.sb(es, "wdw_c", [128, KC, 31], F32)
        L["b_dw_c"] = self.sb(es, "b_dw_c", [128, KC], F32)
        L["aln_g_c"] = self.sb(es, "aln_g_c", [128, KC], F32)
        L["aln_b_c"] = self.sb(es, "aln_b_c", [128, KC], F32)
        L["b_out_f"] = self.sb(es, "b_out_f", [1, D], F32)
        L["b_out_b"] = self.sb(es, "b_out_b", [1, D], BF16)
        L["ones_row_b"] = self.sb(es, "ones_row_b", [1, 128], BF16)
        L["ones_row_f"] = self.sb(es, "ones_row_f", [1, 128], F32)
        self.BIG = self.sb(es, "BIG", [128, 56320], BF16)
        T.dma("sp", lambda: sp.dma_start(out=C["ident_f"][:], in_=I["ident_f"][:, :]), writes=["ident_f"])
        T.dma("sp", lambda: sp.dma_start(out=C["tri_f"][:], in_=I["tri_f"][:, :]), writes=["tri_f"])
        T.dma("sp", lambda: sp.dma_start(out=C["blk_start"][:], in_=I["blk_start"][:, :]), writes=["blk_start"])
        T.dma("sp", lambda: sp.dma_start(out=C["iota_p"][:], in_=I["iota_p"][:, :]), writes=["iota_p"])
        T.dma("sp", lambda: sp.dma_start(out=C["flag"][:], in_=I["flag"][:, :]), writes=["flag"])
        T.dma("sp", lambda: sp.dma_start(out=C["c_col"][:], in_=I["c_col"][:, :]), writes=["c_col"])
        T.op("dve", lambda: dve.tensor_copy(out=C["ident_b"][:], in_=C["ident_f"][:]), reads=["ident_f"], writes=["ident_b"])
        T.op("dve", lambda: dve.memset(C["ones_b"][:], 1.0), writes=["ones_b"])
        T.op("dve", lambda: dve.memset(C["ones_f"][:], 1.0), writes=["ones_f"])
        T.op("dve", lambda: dve.memset(C["eps"][:], EPS), writes=["eps"])
        T.op("dve", lambda: dve.memset(C["kc"][:, 0:1], ALPHA), swrites=["kc"])
        T.op("dve", lambda: dve.memset(C["kc"][:, 1:2], -1.0), swrites=["kc"])
        T.op("dve", lambda: dve.memset(C["kc"][:, 2:3], 1.0 / D), swrites=["kc"])
        T.op("dve", lambda: dve.memset(C["kc"][:, 3:4], -1e30), swrites=["kc"])
        T.op("dve", lambda: dve.memset(L["ones_row_b"][:], 1.0), writes=["ones_row_b"])
        T.op("dve", lambda: dve.memset(L["ones_row_f"][:], 1.0), writes=["ones_row_f"])
        T.op("act", lambda: act.activation(out=C["c_act"][:], in_=C["c_col"][:], func=AF.Silu), reads=["c_col"], writes=["c_act"])
        T.barrier()

        for li, layer in enumerate(self.layers):
            src = I["x_in"] if li == 0 else S["xres"]
            is_last = (li == len(self.layers) - 1)
            phases = [("mods", lambda: self.phase_mods(layer)), ("mixer", lambda: self.phase_mixer(layer, src)),
                      ("route", lambda: self.phase_route_finish(layer)), ("scatter", lambda: self.phase_scatter(layer)),
                      ("experts", lambda: self.phase_experts(layer)), ("combine", lambda: self.phase_combine(layer, is_last))]
            stopped = False
            for pname, pfn in phases:
                pfn()
                if self.stop == pname:
                    stopped = True
                    break
            if stopped:
                if self.stop != "mods":
                    for t_ in range(NTILE):
                        T.dma("sp", lambda: sp.dma_start(out=self.out[t_ * 128:(t_ + 1) * 128, :], in_=S["xres"][t_ * 128:(t_ + 1) * 128, :]))
                break
        T.barrier()
        es.close()

    def mods_gen(self, layer, es, pw=1024, nbuf=2):
        nc, T, I, S, C, L = self.nc, self.T, self.I, self.S, self.C, self.L
        pe, act, dve, pool, sp = nc.tensor, nc.scalar, nc.vector, nc.gpsimd, nc.sync
        aw = self.sb(es, "aw", [128, nbuf, KC, pw], BF16)
        brow = self.sb(es, "brow", [1, nbuf, pw], F32)
        mrow = self.sb(es, "mrow", [1, nbuf, pw], F32)
        nh = pw // 512
        psm = [self.ps(es, "psm%d" % i, [128, 512]) for i in range(2)]
        for n in range(6 * D // pw):
            b = n % nbuf
            T.dma("pool", lambda: pool.dma_start(
                out=aw[:, b, :, :], in_=I["ada_w"][layer, :, n * pw:(n + 1) * pw].rearrange("(k p) f -> p k f", p=128)),
                writes=[("aw", b)])
            T.dma("sp", lambda: sp.dma_start(out=brow[0:1, b, :], in_=I["ada_b"][layer:layer + 1, n * pw:(n + 1) * pw]),
                  writes=[("brow", b)])
            for h in range(nh):
                hb = (n * nh + h) % 2
                for k in range(KC):
                    T.op("pe", lambda: pe.matmul(psm[hb][0:1, :], lhsT=C["c_act"][:, k:k + 1], rhs=aw[:, b, k, h * 512:(h + 1) * 512],
                                                 start=(k == 0), stop=(k == KC - 1)),
                         reads=[("aw", b), "c_act"], writes=[("psm", hb)], signal=(k == KC - 1))
                T.op("dve", lambda: dve.tensor_tensor(out=mrow[0:1, b, h * 512:(h + 1) * 512], in0=psm[hb][0:1, :],
                                                      in1=brow[0:1, b, h * 512:(h + 1) * 512], op=ALU.add),
                     reads=[("psm", hb), ("brow", b)], swrites=[("mrow", b)])
            T.dma("sp", lambda: sp.dma_start(out=S["mods"][layer:layer + 1, n * pw:(n + 1) * pw], in_=mrow[0:1, b, :]),
                  reads=[("mrow", b)], swrites=["mods_d"])
            yield
        self.mods_done = layer

    def phase_mods(self, layer):
        nc, T, I, S, C, L = self.nc, self.T, self.I, self.S, self.C, self.L
        pe, act, dve, pool, sp = nc.tensor, nc.scalar, nc.vector, nc.gpsimd, nc.sync
        if getattr(self, "mods_done", None) != layer:
            with ExitStack() as es:
                for _ in self.mods_gen(layer, es):
                    pass
                T.barrier()
        mods = S["mods"]
        T.dma("sp", lambda: sp.dma_start(out=L["modc"][:], in_=mods[layer, :].rearrange("(v k p) -> p v k", p=128, k=KC),
                                         allow_slow_non_contiguous=True), writes=["modc"])
        T.dma("sp", lambda: sp.dma_start(out=L["gB"][:], in_=mods[layer:layer + 1, 2 * D:3 * D].partition_broadcast(128)), writes=["gB"])
        T.dma("sp", lambda: sp.dma_start(out=L["lngB"][:], in_=I["mix_ln_g"][layer:layer + 1, :].partition_broadcast(128)), writes=["lngB"])
        T.dma("sp", lambda: sp.dma_start(out=L["lnbB"][:], in_=I["mix_ln_b"][layer:layer + 1, :].partition_broadcast(128)), writes=["lnbB"])
        T.dma("sp", lambda: sp.dma_start(out=L["lngc"][:], in_=I["mix_ln_g_c"][layer, :, :]), writes=["lngc"])
        T.dma("sp", lambda: sp.dma_start(out=L["lnbc"][:], in_=I["mix_ln_b_c"][layer, :, :]), writes=["lnbc"])
        T.dma("sp", lambda: sp.dma_start(out=L["rw"][:], in_=I["r_w"][layer, :, :].rearrange("(k p) e -> p k e", p=128)), writes=["rw"])
        T.dma("sp", lambda: sp.dma_start(out=L["rbrow"][:], in_=I["r_b"][layer:layer + 1, :]), writes=["rbrow"])
        T.op("dve", lambda: dve.tensor_scalar(out=L["gB"][:], in0=L["gB"][:], scalar1=1.0, scalar2=None, op0=ALU.add), reads=["gB"], writes=["gB"])
        T.op("dve", lambda: dve.tensor_scalar(out=L["sc1m"][:], in0=L["modc"][:, 1, :], scalar1=1.0, scalar2=None, op0=ALU.add),
             reads=["modc"], writes=["sc1m"])
        T.op("dve", lambda: dve.tensor_scalar(out=L["A2c"][:], in0=L["modc"][:, 4, :], scalar1=1.0, scalar2=None, op0=ALU.add),
             reads=["modc"], writes=["A2c"])
        T.op("dve", lambda: dve.tensor_tensor(out=L["B2c"][:], in0=L["A2c"][:], in1=L["lnbc"][:], op=ALU.mult), reads=["A2c", "lnbc"], writes=["B2c"])
        T.op("dve", lambda: dve.tensor_tensor(out=L["B2c"][:], in0=L["B2c"][:], in1=L["modc"][:, 3, :], op=ALU.add), reads=["B2c", "modc"], writes=["B2c"])
        T.op("dve", lambda: dve.tensor_tensor(out=L["A2c"][:], in0=L["A2c"][:], in1=L["lngc"][:], op=ALU.mult), reads=["A2c", "lngc"], writes=["A2c"])
        T.op("dve", lambda: dve.tensor_tensor(out=L["rwf"][:], in0=L["rw"][:], in1=L["A2c"][:].unsqueeze(2).to_broadcast([128, KC, 36]), op=ALU.mult),
             reads=["rw", "A2c"], writes=["rwf"])
        with ExitStack() as es:
            psb = self.ps(es, "psb", [128, 512])
            for k in range(KC):
                T.op("pe", lambda: pe.matmul(psb[0:1, 0:36], lhsT=L["B2c"][:, k:k + 1], rhs=L["rw"][:, k, :], start=(k == 0), stop=(k == KC - 1)),
                     reads=["B2c", "rw"], writes=["psb"], signal=(k == KC - 1))
            T.op("dve", lambda: dve.tensor_tensor(out=L["rbrow"][:], in0=L["rbrow"][:], in1=psb[0:1, 0:36], op=ALU.add),
                 reads=["psb", "rbrow"], writes=["rbrow"])
            T.barrier()

    def load_mixer_weights(self, layer):
        nc, T, I = self.nc, self.T, self.I
        pool = nc.gpsimd
        is_a = (layer % 2 == 0)
        j_ = layer // 2
        NT_IN = 16 if is_a else 24
        BIG = self.BIG
        w_in = BIG[:, 0:KC * NT_IN * 128].rearrange("p (k f) -> p k f", k=KC)
        o1 = KC * NT_IN * 128
        w_out = BIG[:, o1:o1 + KC * D].rearrange("p (k f) -> p k f", k=KC)
        win_d = I["a_w_in"] if is_a else I["b_w_in"]
        wout_d = I["a_w_out"] if is_a else I["b_w_out"]
        for k in range(KC):
            T.dma("pool", lambda: pool.dma_start(out=w_in[:, k, :], in_=win_d[j_, k * 128:(k + 1) * 128, :]), swrites=["w_in"])
        for k in range(KC):
            T.dma("pool", lambda: pool.dma_start(out=w_out[:, k, :], in_=wout_d[j_, k * 128:(k + 1) * 128, :]), swrites=["w_out"])
        self.mixw_prefetched = layer

    def phase_mixer(self, layer, src):
        nc, T, I, S, C, L, R = self.nc, self.T, self.I, self.S, self.C, self.L, self.R
        pe, act, dve, pool, sp = nc.tensor, nc.scalar, nc.vector, nc.gpsimd, nc.sync
        is_a = (layer % 2 == 0)
        j_ = layer // 2
        BIG = self.BIG
        NT_IN = 16 if is_a else 24
        NTAP = 31 if is_a else 3
        HIST = NTAP - 1
        w_in = BIG[:, 0:KC * NT_IN * 128].rearrange("p (k f) -> p k f", k=KC)
        o1 = KC * NT_IN * 128
        w_out = BIG[:, o1:o1 + KC * D].rearrange("p (k f) -> p k f", k=KC)
        o2 = o1 + KC * D
        diag = BIG[:, o2:o2 + KC * NTAP * 128].rearrange("p (j t m) -> p j t m", j=KC, t=NTAP)
        with ExitStack() as es:
            if getattr(self, "mixw_prefetched", None) != layer:
                self.load_mixer_weights(layer)
            wdw_d = I["a_w_dw_c"] if is_a else I["b_w_dw_c"]
            T.dma("sp", lambda: sp.dma_start(out=L["wdw_c"][:, :, 0:NTAP], in_=wdw_d[j_, :, :, :]), writes=["wdw_c"])
            if is_a:
                T.dma("sp", lambda: sp.dma_start(out=L["b_in_c"][:], in_=I["a_b_in_c"][j_, :, :]), writes=["b_in_c"])
                T.dma("sp", lambda: sp.dma_start(out=L["b_dw_c"][:], in_=I["a_b_dw_c"][j_, :, :]), writes=["b_dw_c"])
                T.dma("sp", lambda: sp.dma_start(out=L["aln_g_c"][:], in_=I["a_ln_g_c"][j_, :, :]), writes=["aln_g_c"])
                T.dma("sp", lambda: sp.dma_start(out=L["aln_b_c"][:], in_=I["a_ln_b_c"][j_, :, :]), writes=["aln_b_c"])
                T.dma("sp", lambda: sp.dma_start(out=L["b_out_f"][:], in_=I["a_b_out"][j_:j_ + 1, :]), writes=["b_out_f"])
                T.op("dve", lambda: dve.tensor_copy(out=L["b_out_b"][:], in_=L["b_out_f"][:]), reads=["b_out_f"], writes=["b_out_b"])
            for j in range(KC):
                T.op("pool", lambda: pool.tensor_tensor(
                    out=diag[:, j, :, :], in0=C["ident_b"][:].unsqueeze(1).to_broadcast([128, NTAP, 128]),
                    in1=L["wdw_c"][:, j, 0:NTAP].unsqueeze(2).to_broadcast([128, NTAP, 128]), op=ALU.mult),
                    reads=["ident_b", "wdw_c"], swrites=["diag"])

            import os
            MS = float(os.environ.get("MIX_STOP", "99"))
            if MS <= 1:
                T.barrier()
                return
            W = WA
            NTW = W // 128
            xt = self.sb(es, "xt", [128, 2, NTW, D], F32)
            hT = self.sb(es, "hT", [128, KC, W], BF16)
            ubuf = self.sb(es, "ubuf", [128, KC, HIST + W], BF16)
            sbf = self.sb(es, "sbf", [128, KC, W], BF16)
            tA = self.sb(es, "tA", [128, 2, W], F32)
            if is_a:
                vbf = self.sb(es, "vbf", [128, KC, W], BF16)
                sq = self.sb(es, "sq", [128, KC, W], BF16)
                mean = self.sb(es, "mean", [128, W], F32)
                rstd = self.sb(es, "rstd", [128, W], F32)
                nmr = self.sb(es, "nmr", [128, W], F32)
                tZ = self.sb(es, "tZ", [128, 2, W], F32)
            else:
                gbs = self.sb(es, "gbs", [128, 2, W], F32)
            r_t = self.sb(es, "r_t", [128, 1, D], F32)
            xn_t = self.sb(es, "xn_t", [128, 2, D], F32)
            xnb_t = self.sb(es, "xnb_t", [128, 1, D], BF16)
            xo_t = self.sb(es, "xo_t", [128, 1, D], F32)
            xnT = self.sb(es, "xnT", [128, KC, 128], F32)
            st6 = self.sb(es, "st6", [128, 4, 3], F32)
            mv = self.sb(es, "mv", [128, 2], F32)
            sm = self.sb(es, "sm", [128, 8], F32)
            lg = self.sb(es, "lg", [128, 36], F32)
            rt = self.sb(es, "rt", [128, 48], F32)
            el = self.sb(es, "el", [128, 2, NEXP], F32)
            pre = self.sb(es, "pre", [128, NEXP], F32)
            tmp32 = self.sb(es, "tmp32", [128, NEXP], F32)
            psT = self.ps(es, "psT", [128, 512])
            psAG = [self.ps(es, "psAG%d" % i, [128, 512]) for i in range(2)]
            psCv = [self.ps(es, "psCv%d" % i, [128, 512]) for i in range(2)]
            psS = self.ps(es, "psS", [128, 512])
            psY = [self.ps(es, "psY%d" % i, [128, 512]) for i in range(2)]

            T.op("dve", lambda: dve.memset(ubuf[:, :, 0:HIST], 0.0), swrites=[("ubuf", j) for j in range(KC)])
            T.op("dve", lambda: dve.memset(R["tot"][:], 0.0), writes=["tot"])

            stiles = [(0, 1)] + [(1 + NTW * s, NTW) for s in range((NTILE - 1) // NTW)]
            assert stiles[-1][0] + stiles[-1][1] == NTILE

            def load(si):
                t0, nt = stiles[si]
                b = si % 2
                T.dma("sp", lambda: sp.dma_start(out=xt[:, b, 0:nt, :], in_=src[t0 * 128:(t0 + nt) * 128, :].rearrange("(t p) d -> p t d", p=128)),
                      writes=[("xt", b)])

            def transposes(si_):
                t0_, nt_ = stiles[si_]
                b_ = si_ % 2
                Wc_ = nt_ * 128
                tbanks = [(psT, "b0"), (psY[0], ("psY", 0)), (psY[1], ("psY", 1))]
                for k in range(KC):
                    pst_, kst_ = tbanks[k % 3]
                    for jt in range(nt_):
                        T.op("pe", lambda: pe.transpose(pst_[:, jt * 128:(jt + 1) * 128], xt[:, b_, jt, k * 128:(k + 1) * 128], C["ident_f"][:]),
                             reads=[("xt", b_), "ident_f"], writes=[kst_], signal=(jt == nt_ - 1))
                    T.op("act", lambda: act.activation(out=hT[:, k, 0:Wc_], in_=pst_[:, 0:Wc_], func=AF.Identity,
                                                       scale=L["sc1m"][:, k:k + 1], bias=L["modc"][:, 0, k:k + 1]),
                         reads=["sc1m", "modc"], writes=[kst_, ("hT", k)])

            load(0)
            load(1)
            transposes(0)
            for si, (t0, nt) in enumerate(stiles):
                b = si % 2
                Wc = nt * 128
                if si >= 1 and si + 1 < len(stiles):
                    load(si + 1)
                hT_keys = [("hT", k) for k in range(KC)]
                if MS <= 2:
                    T.barrier()
                    return
                def inproj(j):
                    par = j % 2
                    kAG = ("bAG", par)
                    kC = ("bC", par)
                    pA = psAG[par][:, 0:Wc]
                    pG = psAG[par][:, 256:256 + Wc]
                    if is_a:
                        for k in range(KC):
                            T.op("pe", lambda: pe.matmul(pA, lhsT=w_in[:, k, j * 128:(j + 1) * 128], rhs=hT[:, k, 0:Wc],
                                                         start=(k == 0), stop=(k == KC - 1)),
                                 reads=hT_keys + ["w_in"], writes=[kAG], signal=False)
                        for k in range(KC):
                            T.op("pe", lambda: pe.matmul(pG, lhsT=w_in[:, k, D + j * 128:D + (j + 1) * 128], rhs=hT[:, k, 0:Wc],
                                                         start=(k == 0), stop=(k == KC - 1)),
                                 reads=hT_keys + ["w_in"], writes=[kAG], signal=(k == KC - 1))
                    else:
                        pV = psCv[par][:, 0:Wc]
                        for part, pst, kk_ in ((0, pA, kAG), (1, pG, kAG), (2, pV, kC)):
                            for k in range(KC):
                                T.op("pe", lambda: pe.matmul(pst, lhsT=w_in[:, k, part * D + j * 128:part * D + (j + 1) * 128],
                                                             rhs=hT[:, k, 0:Wc], start=(k == 0), stop=(k == KC - 1)),
                                     reads=hT_keys + ["w_in"], writes=[kk_], signal=(k == KC - 1 and part >= 1))

                def rest(j):
                    par = j % 2
                    kAG = ("bAG", par)
                    kC = ("bC", par)
                    pA = psAG[par][:, 0:Wc]
                    pG = psAG[par][:, 256:256 + Wc]
                    if is_a:
                        pCv = psCv[par][:, 0:Wc]
                        T.op("act", lambda: act.activation(out=tA[:, par, 0:Wc], in_=pG, func=AF.Sigmoid,
                                                           bias=L["b_in_c"][:, KC + j:KC + j + 1], scale=1.0),
                             reads=["b_in_c"], writes=[kAG, ("tA", par)])
                        T.op("dve", lambda: dve.scalar_tensor_tensor(out=ubuf[:, j, HIST:HIST + Wc], in0=pA,
                                                                     scalar=L["b_in_c"][:, j:j + 1], in1=tA[:, par, 0:Wc],
                                                                     op0=ALU.add, op1=ALU.mult),
                             reads=[("tA", par), "b_in_c"], writes=[kAG, ("ubuf", j)])
                    else:
                        pV = psCv[par][:, 0:Wc]
                        pCv = psCv[par][:, 256:256 + Wc]
                        T.op("act", lambda: act.copy(out=gbs[:, par, 0:Wc], in_=pA), writes=[kAG, ("gbs", par)])
                        T.op("act", lambda: act.copy(out=tA[:, par, 0:Wc], in_=pV), writes=[kC, ("tA", par)])
                        T.op("dve", lambda: dve.tensor_tensor(out=ubuf[:, j, HIST:HIST + Wc], in0=pG, in1=tA[:, par, 0:Wc], op=ALU.mult),
                             reads=[("tA", par)], writes=[kAG, ("ubuf", j)])
                    if si == 0:
                        T.op("dve", lambda: dve.tensor_scalar(out=ubuf[:, j, HIST:HIST + Wc], in0=ubuf[:, j, HIST:HIST + Wc],
                                                              scalar1=C["flag"][:, 0:1], scalar2=None, op0=ALU.mult),
                             reads=["flag"], writes=[("ubuf", j)])
                    return pCv, kC

                def conv_mm(j, pCv, kC):
                    for t in range(NTAP):
                        T.op("pe", lambda: pe.matmul(pCv, lhsT=diag[:, j, t, :], rhs=ubuf[:, j, t:t + Wc],
                                                     start=(t == 0), stop=(t == NTAP - 1)),
                             reads=[("ubuf", j), "diag"], writes=[kC], signal=(t == NTAP - 1))

                def conv_evac(j, pCv, kC):
                    par = j % 2
                    if is_a:
                        T.op("act", lambda: act.activation(out=vbf[:, j, 0:Wc], in_=pCv, func=AF.Identity,
                                                           bias=L["b_dw_c"][:, j:j + 1], scale=1.0),
                             reads=["b_dw_c"], writes=[kC, ("vbf", j)])
                        T.op("act", lambda: act.activation(out=sq[:, j, 0:Wc], in_=pCv, func=AF.Square,
                                                           bias=L["b_dw_c"][:, j:j + 1], scale=1.0),
                             reads=["b_dw_c"], writes=[kC, ("sq", j)])
                    else:
                        T.op("dve", lambda: dve.tensor_tensor(out=sbf[:, j, 0:Wc], in0=pCv, in1=gbs[:, par, 0:Wc], op=ALU.mult),
                             reads=[("gbs", par)], writes=[kC, ("sbf", j)])

                inproj(0)
                cur = rest(0)
                for j in range(KC):
                    pCv_, kC_ = cur
                    if j + 1 < KC and is_a:
                        inproj(j + 1)
                    self.pump(steps=1)
                    conv_mm(j, pCv_, kC_)
                    if j + 1 < KC and not is_a:
                        inproj(j + 1)
                    if j + 1 < KC:
                        cur = rest(j + 1)
                    conv_evac(j, pCv_, kC_)
                    self.pump(steps=1)
                if MS <= 3:
                    T.barrier()
                    return
                T.op("pool", lambda: pool.tensor_copy(out=ubuf[:, :, 0:HIST], in_=ubuf[:, :, Wc:Wc + HIST]),
                     reads=[("ubuf", j) for j in range(KC)], writes=[("ubuf", j) for j in range(KC)])
                if MS <= 3.2:
                    T.barrier()
                    return
                if is_a:
                    for j in range(KC):
                        T.op("pe", lambda: pe.matmul(psS[:, 0:Wc], lhsT=C["ones_b"][:], rhs=vbf[:, j, 0:Wc], start=(j == 0), stop=(j == KC - 1)),
                             reads=[("vbf", j), "ones_b"], writes=["b5"], signal=False)
                    for j in range(KC):
                        T.op("pe", lambda: pe.matmul(psS[:, 256:256 + Wc], lhsT=C["ones_b"][:], rhs=sq[:, j, 0:Wc], start=(j == 0), stop=(j == KC - 1)),
                             reads=[("sq", j), "ones_b"], writes=["b5"], signal=(j == KC - 1))
                    if MS <= 3.4:
                        T.barrier()
                        return
                    T.op("act", lambda: act.activation(out=mean[:, 0:Wc], in_=psS[:, 0:Wc], func=AF.Identity, scale=1.0 / D),
                         writes=["b5", "mean"])
                    if MS <= 3.42:
                        T.barrier()
                        return
                    T.op("act", lambda: act.activation(out=rstd[:, 0:Wc], in_=psS[:, 256:256 + Wc], func=AF.Identity, scale=1.0 / D),
                         writes=["b5", "rstd"])
                    if MS <= 3.44:
                        T.barrier()
                        return
                    if MS <= 3.45:
                        T.barrier()
                        return
                    T.op("dve", lambda: dve.tensor_tensor(out=nmr[:, 0:Wc], in0=mean[:, 0:Wc], in1=mean[:, 0:Wc], op=ALU.mult),
                         reads=["mean"], writes=["nmr"])
                    if MS <= 3.5:
                        T.barrier()
                        return
                    T.op("dve", lambda: dve.tensor_tensor(out=rstd[:, 0:Wc], in0=rstd[:, 0:Wc], in1=nmr[:, 0:Wc], op=ALU.subtract),
                         reads=["rstd", "nmr"], writes=["rstd"])
                    if MS <= 3.6:
                        T.barrier()
                        return
                    T.op("act", lambda: act.activation(out=rstd[:, 0:Wc], in_=rstd[:, 0:Wc], func=AF.Sqrt, bias=C["eps"][:, 0:1], scale=1.0),
                         reads=["rstd", "eps"], writes=["rstd"])
                    T.op("dve", lambda: dve.reciprocal(out=rstd[:, 0:Wc], in_=rstd[:, 0:Wc]), reads=["rstd"], writes=["rstd"])
                    T.op("dve", lambda: dve.scalar_tensor_tensor(out=nmr[:, 0:Wc], in0=mean[:, 0:Wc], scalar=C["kc"][:, 1:2], in1=rstd[:, 0:Wc],
                                                                 op0=ALU.mult, op1=ALU.mult),
                         reads=["mean", "rstd"], writes=["nmr"])
                    if MS <= 3.8:
                        T.barrier()
                        return
                    for j in range(KC):
                        T.op("dve", lambda: dve.tensor_tensor(out=tZ[:, j % 2, 0:Wc], in0=vbf[:, j, 0:Wc], in1=rstd[:, 0:Wc], op=ALU.mult),
                             reads=[("vbf", j), "rstd"], writes=[("tZ", j % 2)])
                        T.op("dve", lambda: dve.tensor_tensor(out=tZ[:, j % 2, 0:Wc], in0=tZ[:, j % 2, 0:Wc], in1=nmr[:, 0:Wc], op=ALU.add),
                             reads=[("tZ", j % 2), "nmr"], writes=[("tZ", j % 2)])
                        T.op("act", lambda: act.activation(out=sbf[:, j, 0:Wc], in_=tZ[:, j % 2, 0:Wc], func=AF.Silu,
                                                           scale=L["aln_g_c"][:, j:j + 1], bias=L["aln_b_c"][:, j:j + 1]),
                             reads=[("tZ", j % 2), "aln_g_c", "aln_b_c"], writes=[("sbf", j)])
                if MS <= 4:
                    T.barrier()
                    return
                for jt in range(nt):
                    tile = t0 + jt
                    tb = tile % 2
                    for hf in range(2):
                        for j in range(KC):
                            last = (j == KC - 1) and not is_a
                            T.op("pe", lambda: pe.matmul(psY[hf][:, :], lhsT=sbf[:, j, jt * 128:(jt + 1) * 128], rhs=w_out[:, j, hf * 512:(hf + 1) * 512],
                                                         start=(j == 0), stop=last),
                                 reads=[("sbf", j), "w_out"], writes=[("psY", hf)], signal=last)
                        if is_a:
                            T.op("pe", lambda: pe.matmul(psY[hf][:, :], lhsT=L["ones_row_b"][0:1, :], rhs=L["b_out_b"][0:1, hf * 512:(hf + 1) * 512],
                                                         start=False, stop=True),
                                 reads=["ones_row_b", "b_out_b"], writes=[("psY", hf)])
                    self.pump(steps=0, upto=tile - 2)
                    hook = None
                    if jt == nt - 1 and si + 1 < len(stiles):
                        hook = (lambda si1=si + 1: transposes(si1))
                    self.epilogue(tile, tb, psY, xt[:, b, jt, :], ("xt", b), r_t, xn_t, xnb_t, xo_t, st6, mv, sm, S["xres"], True, hook=hook)
                    self.pending.append((tile, self.router_gen(tile, tb, xn_t, xnT, psT, psS, lg, rt, el, pre, tmp32)))
            self.pump(drain=True)
            T.barrier()

    def epilogue(self, tile, tb, psY, x_ap, x_key, r_t, xn_t, xnb_t, xo_t, st6, mv, sm, dst, want_xnb, y_sb=None, dst_row0=None, aff="pool", hook=None):
        nc, T, C, L, S = self.nc, self.T, self.C, self.L, self.S
        pe, act, dve, pool, sp = nc.tensor, nc.scalar, nc.vector, nc.gpsimd, nc.sync
        rb = tb % r_t.shape[1]
        ob = tb % xo_t.shape[1]
        r = r_t[:, rb, :]
        if y_sb is None:
            for hf in range(2):
                T.op("dve", lambda: dve.tensor_tensor(out=r[:, hf * 512:(hf + 1) * 512], in0=psY[hf][:, :], in1=L["gB"][:, hf * 512:(hf + 1) * 512], op=ALU.mult),
                     reads=["gB"], writes=[("psY", hf)], swrites=[("r", rb)])
        else:
            T.op("dve", lambda: dve.tensor_tensor(out=r, in0=y_sb[0], in1=L["gB"][:], op=ALU.mult), reads=[y_sb[1], "gB"], swrites=[("r", rb)])
        T.op("dve", lambda: dve.scalar_tensor_tensor(out=r, in0=x_ap, scalar=C["kc"][:, 0:1], in1=r, op0=ALU.mult, op1=ALU.add),
             reads=[x_key, ("r", rb)], writes=[("r", rb)])
        T.op("dve", lambda: dve.bn_stats(out=st6[:, 0:2, :], in_=r[:, 0:512]), reads=[("r", rb)], swrites=["st6"])
        T.op("dve", lambda: dve.bn_stats(out=st6[:, 2:4, :], in_=r[:, 512:1024]), reads=[("r", rb)], swrites=["st6"])
        T.op("dve", lambda: dve.bn_aggr(out=mv[:], in_=st6[:]), reads=["st6"], writes=["mv"])
        if hook is not None:
            hook()
        T.op("act", lambda: act.activation(out=sm[:, 0:1], in_=mv[:, 1:2], func=AF.Sqrt, bias=C["eps"][:, 0:1], scale=1.0), reads=["mv", "eps"], writes=["sm0"])
        T.op("dve", lambda: dve.reciprocal(out=sm[:, 1:2], in_=sm[:, 0:1]), reads=["sm0"], writes=["sm1"])
        T.op("dve", lambda: dve.scalar_tensor_tensor(out=sm[:, 2:3], in0=mv[:, 0:1], scalar=C["kc"][:, 1:2], in1=sm[:, 1:2], op0=ALU.mult, op1=ALU.mult),
             reads=["mv", "sm1"], writes=["sm2"])
        xn = xn_t[:, tb, :]
        if aff == "pool":
            T.op("pool", lambda: pool.tensor_scalar(out=xn, in0=r, scalar1=sm[:, 1:2], scalar2=sm[:, 2:3], op0=ALU.mult, op1=ALU.add),
                 reads=[("r", rb), "sm1", "sm2"], writes=[("xn", tb)])
        else:
            T.op("act", lambda: act.activation(out=xn, in_=r, func=AF.Identity, scale=sm[:, 1:2], bias=sm[:, 2:3]),
                 reads=[("r", rb), "sm1", "sm2"], writes=[("xn", tb)])
        if want_xnb:
            nb = tb % xnb_t.shape[1]
            T.op("pool", lambda: pool.tensor_copy(out=xnb_t[:, nb, :], in_=xn), reads=[("xn", tb)], writes=[("xnb", nb)])
            T.dma("sp", lambda: sp.dma_start(out=S["xnb"][tile * 128:(tile + 1) * 128, :], in_=xnb_t[:, nb, :]),
                  reads=[("xnb", nb)], swrites=["xnb_d"])
        xo = xo_t[:, ob, :]
        an = "dve" if aff == "mix" else "pool"
        ae = self.T.eng[an]
        T.op(an, lambda: ae.tensor_tensor(out=xo, in0=xn, in1=L["lngB"][:], op=ALU.mult), reads=[("xn", tb), "lngB"], writes=[("xo", ob)])
        T.op(an, lambda: ae.tensor_tensor(out=xo, in0=xo, in1=L["lnbB"][:], op=ALU.add), reads=[("xo", ob), "lnbB"], writes=[("xo", ob)])
        row0 = tile * 128 if dst_row0 is None else dst_row0
        T.dma("sp", lambda: sp.dma_start(out=dst[row0:row0 + 128, :], in_=xo), reads=[("xo", ob)], swrites=["xres_d"])

    def router_gen(self, tile, tb, xn_t, xnT, psT, psS, lg, rt, el, pre, tmp32):
        nc, T, C, L, R = self.nc, self.T, self.C, self.L, self.R
        pe, act, dve, pool, sp = nc.tensor, nc.scalar, nc.vector, nc.gpsimd, nc.sync
        xn = xn_t[:, tb, :]
        for rnd in range(2):
            for kk in range(4):
                k = rnd * 4 + kk
                T.op("pe", lambda: pe.transpose(psT[:, kk * 128:(kk + 1) * 128], xn[:, k * 128:(k + 1) * 128], C["ident_f"][:]),
                     reads=[("xn", tb), "ident_f"], writes=["b0"], signal=(kk == 3))
            T.op("act", lambda: act.copy(out=xnT[:, rnd * 4:(rnd + 1) * 4, :], in_=psT[:, :].rearrange("p (k m) -> p k m", k=4)),
                 writes=["b0"], swrites=["xnT"])
            yield
        psL = psS[:, 0:36]
        psP = psS[:, 64:96]
        psQ = psS[:, 128:160]
        for k in range(KC):
            T.op("pe", lambda: pe.matmul(psL, lhsT=xnT[:, k, :], rhs=L["rwf"][:, k, :], start=(k == 0), stop=False),
                 reads=["xnT", "rwf"], writes=["b5"], signal=False)
        T.op("pe", lambda: pe.matmul(psL, lhsT=L["ones_row_f"][0:1, :], rhs=L["rbrow"][0:1, :], start=False, stop=True),
             reads=["ones_row_f", "rbrow"], writes=["b5"])
        T.op("dve", lambda: dve.tensor_copy(out=lg[:], in_=psL), writes=["b5", "lg"])
        yield
        T.op("dve", lambda: dve.tensor_reduce(out=rt[:, 0:1], in_=lg[:, 0:4], axis=AX.X, op=ALU.max), reads=["lg"], writes=["rt0"])
        T.op("dve", lambda: dve.tensor_scalar(out=rt[:, 1:2], in0=rt[:, 0:1], scalar1=-1.0, scalar2=None, op0=ALU.mult), reads=["rt0"], writes=["rt1"])
        T.op("act", lambda: act.activation(out=rt[:, 16:20], in_=lg[:, 0:4], func=AF.Exp, bias=rt[:, 1:2], scale=1.0), reads=["lg", "rt1"], writes=["rt16"])
        T.op("dve", lambda: dve.tensor_reduce(out=rt[:, 2:3], in_=rt[:, 16:20], axis=AX.X, op=ALU.add), reads=["rt16"], writes=["rt2"])
        T.op("dve", lambda: dve.reciprocal(out=rt[:, 3:4], in_=rt[:, 2:3]), reads=["rt2"], writes=["rt3"])
        T.op("dve", lambda: dve.tensor_scalar(out=rt[:, 12:16], in0=lg[:, 0:4], scalar1=rt[:, 0:1], scalar2=None, op0=ALU.is_ge), reads=["lg", "rt0"], writes=["rt12"])
        T.op("dve", lambda: dve.tensor_scalar(out=rt[:, 12:16], in0=rt[:, 12:16], scalar1=-1.0, scalar2=1e30, op0=ALU.add, op1=ALU.mult), reads=["rt12"], writes=["rt12"])
        T.op("dve", lambda: dve.tensor_tensor(out=el[:, 0, :].rearrange("p (g e) -> p g e", g=4), in0=lg[:, 4:36].rearrange("p (g e) -> p g e", g=4),
                                              in1=rt[:, 12:16].unsqueeze(2).to_broadcast([128, 4, 8]), op=ALU.add), reads=["lg", "rt12"], writes=["el0"])
        yield
        T.op("dve", lambda: dve.tensor_reduce(out=rt[:, 4:5], in_=el[:, 0, :], axis=AX.X, op=ALU.max), reads=["el0"], writes=["rt4"])
        oh1 = R["oh1"][:, tile, :]
        oh2 = R["oh2"][:, tile, :]
        T.op("dve", lambda: dve.tensor_scalar(out=oh1, in0=el[:, 0, :], scalar1=rt[:, 4:5], scalar2=None, op0=ALU.is_ge), reads=["el0", "rt4"], writes=["oh1"])
        T.op("dve", lambda: dve.scalar_tensor_tensor(out=el[:, 1, :], in0=oh1, scalar=C["kc"][:, 3:4], in1=el[:, 0, :], op0=ALU.mult, op1=ALU.add),
             reads=["oh1", "el0"], writes=["el1"])
        T.op("dve", lambda: dve.tensor_reduce(out=rt[:, 5:6], in_=el[:, 1, :], axis=AX.X, op=ALU.max), reads=["el1"], writes=["rt5"])
        T.op("dve", lambda: dve.tensor_scalar(out=oh2, in0=el[:, 1, :], scalar1=rt[:, 5:6], scalar2=None, op0=ALU.is_ge), reads=["el1", "rt5"], writes=["oh2"])
        T.op("dve", lambda: dve.tensor_tensor(out=rt[:, 6:7], in0=rt[:, 4:5], in1=rt[:, 5:6], op=ALU.subtract), reads=["rt4", "rt5"], writes=["rt6"])
        T.op("act", lambda: act.activation(out=rt[:, 7:8], in_=rt[:, 6:7], func=AF.Sigmoid), reads=["rt6"], writes=["rt7"])
        w1 = R["w1"][:, tile:tile + 1]
        w2 = R["w2"][:, tile:tile + 1]
        T.op("dve", lambda: dve.tensor_tensor(out=w1, in0=rt[:, 7:8], in1=rt[:, 3:4], op=ALU.mult), reads=["rt7", "rt3"], writes=["w1"])
        T.op("dve", lambda: dve.tensor_tensor(out=w2, in0=rt[:, 3:4], in1=w1, op=ALU.subtract), reads=["rt3", "w1"], writes=["w2"])
        T.op("dve", lambda: dve.tensor_tensor(out=tmp32[:], in0=oh1, in1=oh2, op=ALU.add), reads=["oh1", "oh2"], writes=["sel"])
        yield
        T.op("pe", lambda: pe.matmul(psP, lhsT=C["tri_f"][:], rhs=tmp32[:], start=True, stop=True), reads=["sel", "tri_f"], writes=["b5"], signal=False)
        T.op("pe", lambda: pe.matmul(psQ, lhsT=C["ones_f"][:], rhs=tmp32[:], start=True, stop=True), reads=["sel", "ones_f"], writes=["b5"])
        T.op("dve", lambda: dve.tensor_tensor(out=pre[:], in0=psP, in1=R["tot"][:], op=ALU.add), reads=["tot"], writes=["b5", "pre"])
        T.op("dve", lambda: dve.tensor_tensor(out=R["tot"][:], in0=psQ, in1=R["tot"][:], op=ALU.add), reads=["pre"], writes=["b5", "tot"])
        T.op("dve", lambda: dve.tensor_tensor(out=tmp32[:], in0=pre[:], in1=oh1, op=ALU.mult), reads=["pre", "oh1"], writes=["sel"])
        T.op("dve", lambda: dve.tensor_reduce(out=R["pos1"][:, tile:tile + 1], in_=tmp32[:], axis=AX.X, op=ALU.add), reads=["sel"], writes=["pos1"])
        T.op("dve", lambda: dve.tensor_tensor(out=tmp32[:], in0=pre[:], in1=oh2, op=ALU.mult), reads=["pre", "oh2", "pos1"], writes=["sel"])
        T.op("dve", lambda: dve.tensor_reduce(out=R["pos2"][:, tile:tile + 1], in_=tmp32[:], axis=AX.X, op=ALU.add), reads=["sel"], writes=["pos2"])

    def phase_route_finish(self, layer):
        nc, T, C, L, R, S = self.nc, self.T, self.C, self.L, self.R, self.S
        pe, act, dve, pool, sp = nc.tensor, nc.scalar, nc.vector, nc.gpsimd, nc.sync
        with ExitStack() as es:
            ci = self.sb(es, "ci", [128, NEXP], I32)
            padf = self.sb(es, "padf", [128, NEXP], F32)
            sc = self.sb(es, "sc", [128, 2, NEXP], F32)
            pend = self.sb(es, "pend", [128, NEXP], F32)
            pstart = self.sb(es, "pstart", [128, NEXP], F32)
            big = self.sb(es, "big", [128, NTILE, NEXP], F32)
            dsum = self.sb(es, "dsum", [128, max(NTILE, NBIG)], F32)
            cmp3 = self.sb(es, "cmp3", [128, NBIG, NEXP], F32)
            berow = self.sb(es, "berow", [128, NBIG], F32)
            T.op("dve", lambda: dve.tensor_scalar(out=padf[:], in0=R["tot"][:], scalar1=float(BS - 1) - (BS / 2.0 - 0.5), scalar2=1.0 / BS,
                                                  op0=ALU.add, op1=ALU.mult), writes=["padf"])
            T.op("dve", lambda: dve.tensor_copy(out=ci[:], in_=padf[:]), reads=["padf"], writes=["ci"])
            T.op("dve", lambda: dve.tensor_copy(out=padf[:], in_=ci[:]), reads=["ci"], writes=["padf"])
            T.op("dve", lambda: dve.tensor_scalar(out=padf[:], in0=padf[:], scalar1=float(BS), scalar2=None, op0=ALU.mult), reads=["padf"], writes=["padf"])
            T.op("dve", lambda: dve.tensor_copy(out=sc[:, 0, :], in_=padf[:]), reads=["padf"], writes=[("sc", 0)])
            cur = 0
            for sh in (1, 2, 4, 8, 16):
                nxt = 1 - cur
                T.op("dve", lambda: dve.tensor_copy(out=sc[:, nxt, 0:sh], in_=sc[:, cur, 0:sh]), reads=[("sc", cur)], swrites=[("sc", nxt)])
                T.op("dve", lambda: dve.tensor_tensor(out=sc[:, nxt, sh:NEXP], in0=sc[:, cur, sh:NEXP], in1=sc[:, cur, 0:NEXP - sh], op=ALU.add),
                     reads=[("sc", cur)], swrites=[("sc", nxt)])
                cur = nxt
            T.op("dve", lambda: dve.tensor_copy(out=pend[:], in_=sc[:, cur, :]), reads=[("sc", cur)], writes=["pend"])
            T.op("dve", lambda: dve.tensor_tensor(out=pstart[:], in0=pend[:], in1=padf[:], op=ALU.subtract), reads=["pend", "padf"], writes=["pstart"])
            for nm, ohk, posk, destk in (("a", "oh1", "pos1", "dest1"), ("b", "oh2", "pos2", "dest2")):
                T.op("dve", lambda: dve.tensor_tensor(out=big[:], in0=R[ohk][:], in1=pstart[:].unsqueeze(1).to_broadcast([128, NTILE, NEXP]), op=ALU.mult),
                     reads=["pstart"], writes=["big"])
                T.op("dve", lambda: dve.tensor_reduce(out=dsum[:, 0:NTILE], in_=big[:], axis=AX.X, op=ALU.add), reads=["big"], writes=["dsum"])
                T.op("dve", lambda: dve.tensor_tensor(out=dsum[:, 0:NTILE], in0=dsum[:, 0:NTILE], in1=R[posk][:], op=ALU.add), reads=["dsum"], writes=["dsum"])
                T.op("dve", lambda: dve.tensor_copy(out=R[destk][:], in_=dsum[:, 0:NTILE]), reads=["dsum"], writes=[destk])
            T.op("dve", lambda: dve.tensor_tensor(out=cmp3[:], in0=pend[:].unsqueeze(1).to_broadcast([128, NBIG, NEXP]),
                                                  in1=C["blk_start"][:].unsqueeze(2).to_broadcast([128, NBIG, NEXP]), op=ALU.is_le),
                 reads=["pend", "blk_start"], writes=["cmp3"])
            T.op("dve", lambda: dve.tensor_reduce(out=berow[:], in_=cmp3[:], axis=AX.X, op=ALU.add), reads=["cmp3"], writes=["berow"])
            T.op("dve", lambda: dve.tensor_scalar(out=berow[:], in0=berow[:], scalar1=31.0, scalar2=128.0, op0=ALU.min, op1=ALU.mult), reads=["berow"], writes=["berow"])
            T.op("dve", lambda: dve.tensor_scalar(out=berow[:], in0=berow[:], scalar1=C["iota_p"][:, 0:1], scalar2=None, op0=ALU.add), reads=["berow", "iota_p"], writes=["berow"])
            T.op("dve", lambda: dve.tensor_copy(out=R["widx"][:], in_=berow[:]), reads=["berow"], writes=["widx"])
            if "dbg_route" in self.dbg:
                dr = self.dbg["dbg_route"]
                T.dma("sp", lambda: sp.dma_start(out=dr[:, 0:NTILE], in_=R["dest1"][:]), reads=["dest1"])
                T.dma("sp", lambda: sp.dma_start(out=dr[:, NTILE:2 * NTILE], in_=R["dest2"][:]), reads=["dest2"])
                T.dma("sp", lambda: sp.dma_start(out=dr[:, 2 * NTILE:2 * NTILE + NBIG], in_=R["widx"][:]), reads=["widx"])
                dw = self.dbg["dbg_w"]
                T.dma("sp", lambda: sp.dma_start(out=dw[:, 0:NTILE], in_=R["w1"][:]))
                T.dma("sp", lambda: sp.dma_start(out=dw[:, NTILE:2 * NTILE], in_=R["w2"][:]))
            T.barrier()

    def phase_scatter(self, layer):
        nc, T, C, L, R, S = self.nc, self.T, self.C, self.L, self.R, self.S
        pe, act, dve, pool, sp = nc.tensor, nc.scalar, nc.vector, nc.gpsimd, nc.sync
        with ExitStack() as es:
            xbl = [self.sb(es, "xb_s%d" % i, [128, D], BF16) for i in range(3)]
            for tile in range(NTILE):
                b = tile % 3
                T.dma("sp", lambda: sp.dma_start(out=xbl[b][:, :], in_=S["xnb"][tile * 128:(tile + 1) * 128, :]), writes=[("xb", b)])
                for dk in ("dest1", "dest2"):
                    T.dma("pool", lambda: pool.indirect_dma_start(
                        out=S["xs"][:, :], out_offset=bass.IndirectOffsetOnAxis(ap=R[dk][:, tile:tile + 1], axis=0),
                        in_=xbl[b][:, :], in_offset=None),
                        reads=[("xb", b)], swrites=["xs_d"])
            T.barrier()

    def phase_experts(self, layer):
        nc, T, I, C, L, R, S = self.nc, self.T, self.I, self.C, self.L, self.R, self.S
        pe, act, dve, pool, sp = nc.tensor, nc.scalar, nc.vector, nc.gpsimd, nc.sync
        ESZ = 3 * 4096
        NWB = 4
        wbuf = self.BIG[:, 0:NWB * ESZ].rearrange("p (s f) -> p s f", s=NWB)
        with ExitStack() as es:
            xin = self.sb(es, "xin_e", [128, 3, D], BF16)
            xT = self.sb(es, "xT_e", [128, 2, KC, 128], BF16)
            sg = self.sb(es, "sg_e", [128, 2, DE], F32)
            hid = self.sb(es, "hid_e", [128, 2, DE], BF16)
            hidT = self.sb(es, "hidT_e", [128, 2, 4, 128], BF16)
            yo = self.sb(es, "yo_e", [128, 2, D], F32)
            psX = [self.ps(es, "psX%d" % i, [128, D], BF16) for i in range(2)]
            psGt = self.ps(es, "psGt", [128, 512])
            psUp = self.ps(es, "psUp", [128, 512])
            psH = [self.ps(es, "psH%d" % i, [128, 1024], BF16) for i in range(2)]
            psYe = [self.ps(es, "psYe%d" % i, [128, 512]) for i in range(2)]

            def wload(B):
                wb = B % NWB
                for mi, nm in enumerate(("e_w_gate", "e_w_up", "e_w_down")):
                    for hh in range(2):
                        T.dma("pool", lambda: pool.indirect_dma_start(
                            out=wbuf[:, wb, mi * 4096 + hh * 2048:mi * 4096 + (hh + 1) * 2048], out_offset=None,
                            in_=I["%s_%d_%d" % (nm, layer, hh)][:, :],
                            in_offset=bass.IndirectOffsetOnAxis(ap=R["widx"][:, B:B + 1], axis=0),
                            ),
                            reads=["widx"], swrites=[("wbuf", wb)])

            NSB = NBIG * NSUB

            def load(sbk):
                b3 = sbk % 3
                T.dma("sp", lambda: sp.dma_start(out=xin[:, b3, :], in_=S["xs"][sbk * 128:(sbk + 1) * 128, :]), writes=[("xin", b3)])

            def s1(bk):
                b2, b3 = bk % 2, bk % 3
                for k in range(KC):
                    hb, kk = k // 4, k % 4
                    T.op("pe", lambda: pe.transpose(psX[hb][:, kk * 128:(kk + 1) * 128], xin[:, b3, k * 128:(k + 1) * 128], C["ident_b"][:]),
                         reads=[("xin", b3), "ident_b"], writes=[("psX", hb)], signal=(kk == 3))
                for kk in range(4):
                    k = kk
                    T.op("act", lambda: act.activation(out=xT[:, b2, k, :], in_=psX[0][:, kk * 128:(kk + 1) * 128], func=AF.Identity,
                                                       scale=L["A2c"][:, k:k + 1], bias=L["B2c"][:, k:k + 1]),
                         reads=["A2c", "B2c"], writes=[("psX", 0)], swrites=[("xT", b2)])
                    k2 = 4 + kk
                    T.op("dve", lambda: dve.tensor_scalar(out=xT[:, b2, k2, :], in0=psX[1][:, kk * 128:(kk + 1) * 128],
                                                          scalar1=L["A2c"][:, k2:k2 + 1], scalar2=L["B2c"][:, k2:k2 + 1], op0=ALU.mult, op1=ALU.add),
                         reads=["A2c", "B2c"], writes=[("psX", 1)], swrites=[("xT", b2)])
                if bk + 3 < NSB:
                    load(bk + 3)

            def s2(bk):
                b2 = bk % 2
                wb = (bk // NSUB) % NWB
                w = wbuf[:, wb, :]
                for nm, pst, off in (("g", psGt, 0), ("u", psUp, 4096)):
                    for k in range(KC):
                        T.op("pe", lambda: pe.matmul(pst[:, :], lhsT=xT[:, b2, k, :], rhs=w[:, off + k * 512:off + (k + 1) * 512],
                                                     start=(k == 0), stop=(k == KC - 1)),
                             reads=[("xT", b2), ("wbuf", wb)], writes=["ps_" + nm], signal=(k == KC - 1))
                T.op("act", lambda: act.activation(out=sg[:, b2, :], in_=psGt[:, :], func=AF.Silu), writes=["ps_g", ("sg", b2)])
                T.op("dve", lambda: dve.tensor_tensor(out=hid[:, b2, :], in0=psUp[:, :], in1=sg[:, b2, :], op=ALU.mult),
                     reads=[("sg", b2)], writes=["ps_u", ("hid", b2)])

            def s3(bk):
                b2 = bk % 2
                for jc in range(4):
                    T.op("pe", lambda: pe.transpose(psH[b2][:, jc * 128:(jc + 1) * 128], hid[:, b2, jc * 128:(jc + 1) * 128], C["ident_b"][:]),
                         reads=[("hid", b2), "ident_b"], writes=[("psH", b2)], signal=(jc == 3))
                T.op("dve", lambda: dve.tensor_copy(out=hidT[:, b2, :, :], in_=psH[b2][:, 0:512].rearrange("p (j m) -> p j m", j=4)),
                     writes=[("psH", b2), ("hidT", b2)])

            def s4(bk):
                b2 = bk % 2
                wb = (bk // NSUB) % NWB
                w = wbuf[:, wb, :]
                for hf in range(2):
                    for jc in range(4):
                        T.op("pe", lambda: pe.matmul(psYe[hf][:, :], lhsT=hidT[:, b2, jc, :],
                                                     rhs=w[:, 8192 + jc * 1024 + hf * 512:8192 + jc * 1024 + (hf + 1) * 512],
                                                     start=(jc == 0), stop=(jc == 3)),
                             reads=[("hidT", b2), ("wbuf", wb)], writes=[("psYe", hf)], signal=(jc == 3))
                T.op("act", lambda: act.copy(out=yo[:, b2, 0:512], in_=psYe[0][:, :]), writes=[("psYe", 0)], swrites=[("yo", b2)])
                T.op("dve", lambda: dve.tensor_copy(out=yo[:, b2, 512:1024], in_=psYe[1][:, :]), writes=[("psYe", 1)], swrites=[("yo", b2)])
                T.dma("sp", lambda: sp.dma_start(out=S["yb"][bk * 128:(bk + 1) * 128, :], in_=yo[:, b2, :]), reads=[("yo", b2)], swrites=["yb_d"])
                if bk % NSUB == NSUB - 1:
                    Bn = bk // NSUB + NWB
                    if Bn < NBIG:
                        wload(Bn)

            for B0 in range(NWB):
                wload(B0)
            load(0)
            load(1)
            load(2)
            s1(0)
            s2(0)
            if NSB > 1:
                s1(1)
            for bk in range(NSB):
                s3(bk)
                if bk + 1 < NSB:
                    s2(bk + 1)
                if bk + 2 < NSB:
                    s1(bk + 2)
                s4(bk)
            T.barrier()

    def phase_combine(self, layer, is_last):
        nc, T, I, C, L, R, S = self.nc, self.T, self.I, self.C, self.L, self.R, self.S
        pe, act, dve, pool, sp = nc.tensor, nc.scalar, nc.vector, nc.gpsimd, nc.sync
        mods = S["mods"]
        T.dma("sp", lambda: sp.dma_start(out=L["gB"][:], in_=mods[layer:layer + 1, 5 * D:6 * D].partition_broadcast(128)), writes=["gB"])
        T.dma("sp", lambda: sp.dma_start(out=L["lngB"][:], in_=I["ffn_ln_g"][layer:layer + 1, :].partition_broadcast(128)), writes=["lngB"])
        T.dma("sp", lambda: sp.dma_start(out=L["lnbB"][:], in_=I["ffn_ln_b"][layer:layer + 1, :].partition_broadcast(128)), writes=["lnbB"])
        T.op("dve", lambda: dve.tensor_scalar(out=L["gB"][:], in0=L["gB"][:], scalar1=1.0, scalar2=None, op0=ALU.add), reads=["gB"], writes=["gB"])
        if not is_last and (layer + 1) in self.layers:
            self.load_mixer_weights(layer + 1)
        with ExitStack() as es:
            y1l = [self.sb(es, "y1_c%d" % i, [128, D], F32) for i in range(2)]
            y2l = [self.sb(es, "y2_c%d" % i, [128, D], F32) for i in range(2)]
            xm = self.sb(es, "xm_c", [128, 2, D], F32)
            r_t = self.sb(es, "r_c", [128, 2, D], F32)
            xn_t = self.sb(es, "xn_c", [128, 2, D], F32)
            xo_t = self.sb(es, "xo_c", [128, 2, D], F32)
            st6 = self.sb(es, "st6_c", [128, 4, 3], F32)
            mv = self.sb(es, "mv_c", [128, 2], F32)
            sm = self.sb(es, "sm_c", [128, 8], F32)
            final = is_last and self.last
            mg = None
            if not is_last and (layer + 1) in self.layers:
                mg = self.mods_gen(layer + 1, es, pw=512, nbuf=1)
            for tile in range(NTILE):
                if mg is not None and tile % 2 == 1:
                    try:
                        next(mg)
                    except StopIteration:
                        mg = None
                if final and tile == 0:
                    continue
                tb = tile % 2
                T.dma("sp", lambda: sp.dma_start(out=xm[:, tb, :], in_=S["xres"][tile * 128:(tile + 1) * 128, :]), writes=[("xm", tb)])
                y1 = y1l[tb]
                y2 = y2l[tb]
                for yk, dk in ((y1, "dest1"), (y2, "dest2")):
                    T.dma("pool", lambda: pool.indirect_dma_start(
                        out=yk[:, :], out_offset=None, in_=S["yb"][:, :],
                        in_offset=bass.IndirectOffsetOnAxis(ap=R[dk][:, tile:tile + 1], axis=0),
                        ), writes=[(dk + "y", tb)])
                T.op("act", lambda: act.activation(out=y1[:, :], in_=y1[:, :], func=AF.Identity, scale=R["w1"][:, tile:tile + 1]),
                     reads=[("dest1y", tb)], writes=[("dest1y", tb)])
                T.op("dve", lambda: dve.scalar_tensor_tensor(out=y1[:, :], in0=y2[:, :], scalar=R["w2"][:, tile:tile + 1], in1=y1[:, :],
                                                              op0=ALU.mult, op1=ALU.add),
                     reads=[("dest1y", tb), ("dest2y", tb)], writes=[("dest1y", tb)])
                if final:
                    self.epilogue(tile, tb, None, xm[:, tb, :], ("xm", tb), r_t, xn_t, None, xo_t, st6, mv, sm, self.out, False,
                                  y_sb=(y1[:, :], ("dest1y", tb)), dst_row0=(tile - 1) * 128, aff="mix")
                else:
                    dst = S["xres"] if not is_last else self.out
                    self.epilogue(tile, tb, None, xm[:, tb, :], ("xm", tb), r_t, xn_t, None, xo_t, st6, mv, sm, dst, False,
                                  y_sb=(y1[:, :], ("dest1y", tb)), aff="mix")
            if mg is not None:
                for _ in mg:
                    pass
            T.barrier()


def _core_inputs(inputs, core):
    b, half = core // 2, core % 2
    x = inputs["x"]
    xin = np.zeros((NTOK, D), np.float32)
    if half == 1:
        xin[:] = x[b, NOWN - HALO:2 * NOWN]
    else:
        xin[HALO:] = x[b, 0:NOWN]
    return {
        "x_in": xin,
        "flag": np.full((128, 1), float(half), np.float32),
        "c_col": np.ascontiguousarray(inputs["c"][b].reshape(KC, 128).T),
    }


def _shared_inputs(inputs):
    f = lambda a: np.ascontiguousarray(np.asarray(a, dtype=np.float32))
    col = lambda a: np.ascontiguousarray(a.reshape(a.shape[0], -1, 128).transpose(0, 2, 1))
    sh = {
        "ident_f": np.eye(128, dtype=np.float32),
        "tri_f": np.triu(np.ones((128, 128), np.float32), 1),
        "blk_start": np.ascontiguousarray(np.broadcast_to((np.arange(NBIG, dtype=np.float32) * BS)[None, :], (128, NBIG))),
        "iota_p": np.arange(128, dtype=np.float32).reshape(128, 1),
        "ada_w": f(inputs["ada_w"]), "ada_b": f(inputs["ada_b"]),
        "a_w_in": f(inputs["a_w_in"]), "a_b_in_c": col(f(inputs["a_b_in"])),
        "a_w_dw_c": np.ascontiguousarray(f(inputs["a_w_dw"]).reshape(2, 31, KC, 128).transpose(0, 3, 2, 1)),
        "a_b_dw_c": col(f(inputs["a_b_dw"])), "a_ln_g_c": col(f(inputs["a_ln_g"])), "a_ln_b_c": col(f(inputs["a_ln_b"])),
        "a_w_out": f(inputs["a_w_out"]), "a_b_out": f(inputs["a_b_out"]),
        "b_w_in": f(inputs["b_w_in"]),
        "b_w_dw_c": np.ascontiguousarray(f(inputs["b_w_dw"]).reshape(2, 3, KC, 128).transpose(0, 3, 2, 1)),
        "b_w_out": f(inputs["b_w_out"]),
        "mix_ln_g": f(inputs["mix_ln_g"]), "mix_ln_b": f(inputs["mix_ln_b"]),
        "ffn_ln_g": f(inputs["ffn_ln_g"]), "ffn_ln_b": f(inputs["ffn_ln_b"]),
        "mix_ln_g_c": col(f(inputs["mix_ln_g"])), "mix_ln_b_c": col(f(inputs["mix_ln_b"])),
        "r_w": np.ascontiguousarray(np.concatenate([f(inputs["r_w_group"]), f(inputs["r_w_expert"])], axis=2)),
        "r_b": np.ascontiguousarray(np.concatenate([f(inputs["r_b_group"]), f(inputs["r_b_expert"])], axis=1)),
    }
    for nm, kk in (("e_w_gate", KC), ("e_w_up", KC), ("e_w_down", 4)):
        a = f(inputs[nm])
        a = a.reshape(DEPTH, NEXP, kk, 128, a.shape[-1]).transpose(0, 1, 3, 2, 4).reshape(DEPTH, NEXP * 128, 4096)
        for l_ in range(DEPTH):
            for hh in range(2):
                sh["%s_%d_%d" % (nm, l_, hh)] = np.ascontiguousarray(a[l_, :, hh * 2048:(hh + 1) * 2048])
    return sh


_PROG_CACHE = {}


def _get_prog(layers, first, last, debug=()):
    key = (tuple(layers), first, last, tuple(debug))
    if key not in _PROG_CACHE:
        _PROG_CACHE[key] = Prog(layers, first, last, debug)
    return _PROG_CACHE[key]


def kernel(**inputs):
    shared = _shared_inputs(inputs)
    prog = _get_prog([0, 1, 2, 3], True, True)
    in_maps = []
    for c in range(NCORES):
        m = dict(shared)
        m.update(_core_inputs(inputs, c))
        in_maps.append(m)
    res = run_bass_kernel_spmd(prog.nc, in_maps, core_ids=list(range(NCORES)))
    out = np.zeros((4, 2 * NOWN, D), np.float32)
    for c in range(NCORES):
        b, half = c // 2, c % 2
        out[b, half * NOWN:(half + 1) * NOWN] = res.results[c]["out"]
    return out
```

```python
import numpy as np
from contextlib import ExitStack
import concourse.bass as bass
import concourse.mybir as mybir
from concourse.bass_utils import run_bass_kernel_spmd

F32 = mybir.dt.float32
BF16 = mybir.dt.bfloat16
I32 = mybir.dt.int32
ALU = mybir.AluOpType
AF = mybir.ActivationFunctionType
AX = mybir.AxisListType

NCORES = 8
D = 1024
KC = 8
HALO = 128
NOWN = 4096
NTOK = NOWN + HALO
NTILE = NTOK // 128
NEXP = 32
DE = 512
BS = 384
NSUB = BS // 128
NBIG = (2 * NTOK) // BS + NEXP
NSLOT = NBIG * BS
DEPTH = 4
ALPHA = (2.0 * DEPTH) ** 0.25
EPS = 1e-5
WA = 256
ENGS = ("pe", "act", "dve", "pool", "sp")

_names = {}


def sem_name(sem):
    k = id(sem)
    if k not in _names:
        _names[k] = "sem%d" % len(_names)
    return _names[k]


class Tracker:
    def __init__(self, nc, es, n_dma_sems=24):
        self.nc = nc
        self.eng = {"pe": nc.tensor, "act": nc.scalar, "dve": nc.vector, "pool": nc.gpsimd, "sp": nc.sync}
        self.psem = {e: es.enter_context(nc.semaphore("prog_" + e)) for e in ("pe", "act", "dve", "pool")}
        self.pcount = {e: 0 for e in self.psem}
        self.dsem = {}
        for q in ("sp", "pool"):
            self.dsem[q] = [[es.enter_context(nc.semaphore(f"dma_{q}{i}")), 0] for i in range(n_dma_sems)]
        self.drr = {q: 0 for q in self.dsem}
        self.waited = {}
        self.writers = {}
        self.readers = {}

    def _wait(self, e, ev):
        sem, name, val = ev
        k = (e, name)
        if self.waited.get(k, 0) >= val:
            return
        self.waited[k] = val
        self.eng[e].wait_ge(sem, val)

    def _deps(self, reads, writes, swrites):
        deps = []
        for k in reads:
            deps += self.writers.get(k, [])
        for k in writes:
            deps += self.writers.get(k, []) + self.readers.get(k, [])
        for k in swrites:
            deps += self.readers.get(k, [])
        return deps

    def _record(self, ev, reads, writes, swrites):
        for k in reads:
            self.readers.setdefault(k, []).append(ev)
        for k in writes:
            self.writers[k] = [ev]
            self.readers[k] = []
        for k in swrites:
            self.writers.setdefault(k, []).append(ev)

    def op(self, e, fn, reads=(), writes=(), swrites=(), signal=True, extra_deps=()):
        skip_same = (e == "pe")
        for ev in list(self._deps(reads, writes, swrites)) + list(extra_deps):
            if skip_same and ev[1] == "prog_pe":
                continue
            self._wait(e, ev)
        ins = fn()
        if signal:
            self.pcount[e] += 1
            ins.then_inc(self.psem[e], 1)
        ev = (self.psem[e], "prog_" + e, self.pcount[e] if signal else self.pcount[e] + 1)
        self._record(ev, reads, writes, swrites)
        return ev

    def dma(self, q, fn, reads=(), writes=(), swrites=(), extra_deps=()):
        for ev in list(self._deps(reads, writes, swrites)) + list(extra_deps):
            self._wait(q, ev)
        slot = self.dsem[q][self.drr[q] % len(self.dsem[q])]
        self.drr[q] += 1
        sem, cnt = slot
        name = sem_name(sem)
        if cnt > 0:
            self._wait(q, (sem, name, cnt))
        ins = fn()
        slot[1] = cnt + 16
        ins.then_inc(sem, 16)
        ev = (sem, name, cnt + 16)
        self._record(ev, reads, writes, swrites)
        return ev

    def all_events(self):
        evs = []
        for e, s in self.psem.items():
            if self.pcount[e] > 0:
                evs.append((s, "prog_" + e, self.pcount[e]))
        for q, lst in self.dsem.items():
            for sem, cnt in lst:
                if cnt > 0:
                    evs.append((sem, sem_name(sem), cnt))
        return evs

    def barrier(self, engines=ENGS):
        evs = self.all_events()
        for e in engines:
            for ev in evs:
                self._wait(e, ev)
        self.writers.clear()
        self.readers.clear()


class Prog:
    def __init__(self, layers, first, last, debug=(), stop=None):
        self.stop = stop
        self.layers = list(layers)
        self.first = first
        self.last = last
        self.debug = debug
        self.pending = []
        self.nc = bass.Bass("TRN2", target_bir_lowering=False)
        self.es = ExitStack()
        self.build()

    def pump(self, steps=2, drain=False, upto=None):
        n = 0
        while self.pending:
            tile, g = self.pending[0]
            if not drain and not (upto is not None and tile <= upto) and n >= steps:
                break
            try:
                next(g)
                n += 1
            except StopIteration:
                self.pending.pop(0)

    def din(self, name, shape, dt=F32):
        return self.nc.dram_tensor(name, list(shape), dt, kind="ExternalInput").ap()

    def dscr(self, name, shape, dt=F32):
        return self.nc.dram_tensor(name, list(shape), dt, kind="Internal").ap()

    def sb(self, es, name, shape, dt):
        self._uid = getattr(self, "_uid", 0) + 1
        return es.enter_context(self.nc.sbuf_tensor("s%d_%s" % (self._uid, name), list(shape), dt))

    def ps(self, es, name, shape, dt=F32):
        self._uid = getattr(self, "_uid", 0) + 1
        return es.enter_context(self.nc.psum_tensor("p%d_%s" % (self._uid, name), list(shape), dt))

    def build(self):
        nc, es = self.nc, self.es
        T = self.T = Tracker(nc, es)
        pe, act, dve, pool, sp = nc.tensor, nc.scalar, nc.vector, nc.gpsimd, nc.sync
        I = self.I = {}
        I["x_in"] = self.din("x_in", [NTOK, D])
        I["flag"] = self.din("flag", [128, 1])
        I["c_col"] = self.din("c_col", [128, KC])
        I["ident_f"] = self.din("ident_f", [128, 128])
        I["tri_f"] = self.din("tri_f", [128, 128])
        I["blk_start"] = self.din("blk_start", [128, NBIG])
        I["iota_p"] = self.din("iota_p", [128, 1])
        I["ada_w"] = self.din("ada_w", [DEPTH, D, 6 * D])
        I["ada_b"] = self.din("ada_b", [DEPTH, 6 * D])
        I["a_w_in"] = self.din("a_w_in", [2, D, 2 * D])
        I["a_b_in_c"] = self.din("a_b_in_c", [2, 128, 16])
        I["a_w_dw_c"] = self.din("a_w_dw_c", [2, 128, KC, 31])
        I["a_b_dw_c"] = self.din("a_b_dw_c", [2, 128, KC])
        I["a_ln_g_c"] = self.din("a_ln_g_c", [2, 128, KC])
        I["a_ln_b_c"] = self.din("a_ln_b_c", [2, 128, KC])
        I["a_w_out"] = self.din("a_w_out", [2, D, D])
        I["a_b_out"] = self.din("a_b_out", [2, D])
        I["b_w_in"] = self.din("b_w_in", [2, D, 3 * D])
        I["b_w_dw_c"] = self.din("b_w_dw_c", [2, 128, KC, 3])
        I["b_w_out"] = self.din("b_w_out", [2, D, D])
        I["mix_ln_g"] = self.din("mix_ln_g", [DEPTH, D])
        I["mix_ln_b"] = self.din("mix_ln_b", [DEPTH, D])
        I["ffn_ln_g"] = self.din("ffn_ln_g", [DEPTH, D])
        I["ffn_ln_b"] = self.din("ffn_ln_b", [DEPTH, D])
        I["mix_ln_g_c"] = self.din("mix_ln_g_c", [DEPTH, 128, KC])
        I["mix_ln_b_c"] = self.din("mix_ln_b_c", [DEPTH, 128, KC])
        I["r_w"] = self.din("r_w", [DEPTH, D, 36])
        I["r_b"] = self.din("r_b", [DEPTH, 36])
        if self.stop not in ("mods", "mixer", "route", "scatter"):
            for nm in ("e_w_gate", "e_w_up", "e_w_down"):
                for l_ in self.layers:
                    for hh in range(2):
                        key = "%s_%d_%d" % (nm, l_, hh)
                        I[key] = self.din(key, [NEXP * 128, 2048])
        self.out = nc.dram_tensor("out", [NTOK if not self.last else NOWN, D], F32, kind="ExternalOutput").ap()
        self.dbg = {}
        for name, shape, dt in self.debug:
            self.dbg[name] = nc.dram_tensor(name, list(shape), dt, kind="ExternalOutput").ap()

        S = self.S = {}
        S["xres"] = self.dscr("xres", [NTOK, D])
        S["xnb"] = self.dscr("xnb", [NTOK, D], BF16)
        S["xs"] = self.dscr("xs", [NSLOT, D], BF16)
        S["yb"] = self.dscr("yb", [NSLOT, D])
        S["mods"] = self.dscr("mods", [DEPTH, 6 * D])

        C = self.C = {}
        C["ident_f"] = self.sb(es, "ident_f", [128, 128], F32)
        C["ident_b"] = self.sb(es, "ident_b", [128, 128], BF16)
        C["ones_b"] = self.sb(es, "ones_b", [128, 128], BF16)
        C["ones_f"] = self.sb(es, "ones_f", [128, 128], F32)
        C["tri_f"] = self.sb(es, "tri_f", [128, 128], F32)
        C["blk_start"] = self.sb(es, "blk_start", [128, NBIG], F32)
        C["iota_p"] = self.sb(es, "iota_p", [128, 1], F32)
        C["flag"] = self.sb(es, "flag", [128, 1], F32)
        C["eps"] = self.sb(es, "eps", [128, 1], F32)
        C["c_col"] = self.sb(es, "c_col", [128, KC], F32)
        C["c_act"] = self.sb(es, "c_act", [128, KC], BF16)
        C["dummy"] = self.sb(es, "dummy", [128, 1], F32)
        C["kc"] = self.sb(es, "kc", [128, 4], F32)
        R = self.R = {}
        R["oh1"] = self.sb(es, "oh1", [128, NTILE, NEXP], BF16)
        R["oh2"] = self.sb(es, "oh2", [128, NTILE, NEXP], BF16)
        R["pos1"] = self.sb(es, "pos1", [128, NTILE], F32)
        R["pos2"] = self.sb(es, "pos2", [128, NTILE], F32)
        R["w1"] = self.sb(es, "w1", [128, NTILE], F32)
        R["w2"] = self.sb(es, "w2", [128, NTILE], F32)
        R["dest1"] = self.sb(es, "dest1", [128, NTILE], I32)
        R["dest2"] = self.sb(es, "dest2", [128, NTILE], I32)
        R["tot"] = self.sb(es, "tot", [128, NEXP], F32)
        R["widx"] = self.sb(es, "widx", [128, NBIG], I32)
        L = self.L = {}
        L["gB"] = self.sb(es, "gB", [128, D], F32)
        L["lngB"] = self.sb(es, "lngB", [128, D], F32)
        L["lnbB"] = self.sb(es, "lnbB", [128, D], F32)
        L["modc"] = self.sb(es, "modc", [128, 6, KC], F32)
        L["sc1m"] = self.sb(es, "sc1m", [128, KC], F32)
        L["A2c"] = self.sb(es, "A2c", [128, KC], F32)
        L["B2c"] = self.sb(es, "B2c", [128, KC], F32)
        L["lngc"] = self.sb(es, "lngc", [128, KC], F32)
        L["lnbc"] = self.sb(es, "lnbc", [128, KC], F32)
        L["rw"] = self.sb(es, "rw", [128, KC, 36], F32)
        L["rwf"] = self.sb(es, "rwf", [128, KC, 36], F32)
        L["rbrow"] = self.sb(es, "rbrow", [1, 36], F32)
        L["b_in_c"] = self.sb(es, "b_in_c", [128, 16], F32)
        L["wdw_c"] = self.sb(es, "wdw_c", [128, KC, 31], F32)
        L["b_dw_c"] = self.sb(es, "b_dw_c", [128, KC], F32)
        L["aln_g_c"] = self.sb(es, "aln_g_c", [128, KC], F32)
        L["aln_b_c"] = self.sb(es, "aln_b_c", [128, KC], F32)
        L["b_out_f"] = self.sb(es, "b_out_f", [1, D], F32)
        L["b_out_b"] = self.sb(es, "b_out_b", [1, D], BF16)
        L["ones_row_b"] = self.sb(es, "ones_row_b", [1, 128], BF16)
        L["ones_row_f"] = self.sb(es, "ones_row_f", [1, 128], F32)
        self.BIG = self.sb(es, "BIG", [128, 56320], BF16)
        T.dma("sp", lambda: sp.dma_start(out=C["ident_f"][:], in_=I["ident_f"][:, :]), writes=["ident_f"])
        T.dma("sp", lambda: sp.dma_start(out=C["tri_f"][:], in_=I["tri_f"][:, :]), writes=["tri_f"])
        T.dma("sp", lambda: sp.dma_start(out=C["blk_start"][:], in_=I["blk_start"][:, :]), writes=["blk_start"])
        T.dma("sp", lambda: sp.dma_start(out=C["iota_p"][:], in_=I["iota_p"][:, :]), writes=["iota_p"])
        T.dma("sp", lambda: sp.dma_start(out=C["flag"][:], in_=I["flag"][:, :]), writes=["flag"])
        T.dma("sp", lambda: sp.dma_start(out=C["c_col"][:], in_=I["c_col"][:, :]), writes=["c_col"])
        T.op("dve", lambda: dve.tensor_copy(out=C["ident_b"][:], in_=C["ident_f"][:]), reads=["ident_f"], writes=["ident_b"])
        T.op("dve", lambda: dve.memset(C["ones_b"][:], 1.0), writes=["ones_b"])
        T.op("dve", lambda: dve.memset(C["ones_f"][:], 1.0), writes=["ones_f"])
        T.op("dve", lambda: dve.memset(C["eps"][:], EPS), writes=["eps"])
        T.op("dve", lambda: dve.memset(C["kc"][:, 0:1], ALPHA), swrites=["kc"])
        T.op("dve", lambda: dve.memset(C["kc"][:, 1:2], -1.0), swrites=["kc"])
        T.op("dve", lambda: dve.memset(C["kc"][:, 2:3], 1.0 / D), swrites=["kc"])
        T.op("dve", lambda: dve.memset(C["kc"][:, 3:4], -1e30), swrites=["kc"])
        T.op("dve", lambda: dve.memset(L["ones_row_b"][:], 1.0), writes=["ones_row_b"])
        T.op("dve", lambda: dve.memset(L["ones_row_f"][:], 1.0), writes=["ones_row_f"])
        T.op("act", lambda: act.activation(out=C["c_act"][:], in_=C["c_col"][:], func=AF.Silu), reads=["c_col"], writes=["c_act"])
        T.barrier()

        for li, layer in enumerate(self.layers):
            src = I["x_in"] if li == 0 else S["xres"]
            is_last = (li == len(self.layers) - 1)
            phases = [("mods", lambda: self.phase_mods(layer)), ("mixer", lambda: self.phase_mixer(layer, src)),
                      ("route", lambda: self.phase_route_finish(layer)), ("scatter", lambda: self.phase_scatter(layer)),
                      ("experts", lambda: self.phase_experts(layer)), ("combine", lambda: self.phase_combine(layer, is_last))]
            stopped = False
            for pname, pfn in phases:
                pfn()
                if self.stop == pname:
                    stopped = True
                    break
            if stopped:
                if self.stop != "mods":
                    for t_ in range(NTILE):
                        T.dma("sp", lambda: sp.dma_start(out=self.out[t_ * 128:(t_ + 1) * 128, :], in_=S["xres"][t_ * 128:(t_ + 1) * 128, :]))
                break
        T.barrier()
        es.close()

    def mods_gen(self, layer, es, pw=1024, nbuf=2):
        nc, T, I, S, C, L = self.nc, self.T, self.I, self.S, self.C, self.L
        pe, act, dve, pool, sp = nc.tensor, nc.scalar, nc.vector, nc.gpsimd, nc.sync
        aw = self.sb(es, "aw", [128, nbuf, KC, pw], BF16)
        brow = self.sb(es, "brow", [1, nbuf, pw], F32)
        mrow = self.sb(es, "mrow", [1, nbuf, pw], F32)
        nh = pw // 512
        psm = [self.ps(es, "psm%d" % i, [128, 512]) for i in range(2)]
        for n in range(6 * D // pw):
            b = n % nbuf
            T.dma("pool", lambda: pool.dma_start(
                out=aw[:, b, :, :], in_=I["ada_w"][layer, :, n * pw:(n + 1) * pw].rearrange("(k p) f -> p k f", p=128)),
                writes=[("aw", b)])
            T.dma("sp", lambda: sp.dma_start(out=brow[0:1, b, :], in_=I["ada_b"][layer:layer + 1, n * pw:(n + 1) * pw]),
                  writes=[("brow", b)])
            for h in range(nh):
                hb = (n * nh + h) % 2
                for k in range(KC):
                    T.op("pe", lambda: pe.matmul(psm[hb][0:1, :], lhsT=C["c_act"][:, k:k + 1], rhs=aw[:, b, k, h * 512:(h + 1) * 512],
                                                 start=(k == 0), stop=(k == KC - 1)),
                         reads=[("aw", b), "c_act"], writes=[("psm", hb)], signal=(k == KC - 1))
                T.op("dve", lambda: dve.tensor_tensor(out=mrow[0:1, b, h * 512:(h + 1) * 512], in0=psm[hb][0:1, :],
                                                      in1=brow[0:1, b, h * 512:(h + 1) * 512], op=ALU.add),
                     reads=[("psm", hb), ("brow", b)], swrites=[("mrow", b)])
            T.dma("sp", lambda: sp.dma_start(out=S["mods"][layer:layer + 1, n * pw:(n + 1) * pw], in_=mrow[0:1, b, :]),
                  reads=[("mrow", b)], swrites=["mods_d"])
            yield
        self.mods_done = layer

    def phase_mods(self, layer):
        nc, T, I, S, C, L = self.nc, self.T, self.I, self.S, self.C, self.L
        pe, act, dve, pool, sp = nc.tensor, nc.scalar, nc.vector, nc.gpsimd, nc.sync
        if getattr(self, "mods_done", None) != layer:
            with ExitStack() as es:
                for _ in self.mods_gen(layer, es):
                    pass
                T.barrier()
        mods = S["mods"]
        T.dma("sp", lambda: sp.dma_start(out=L["modc"][:], in_=mods[layer, :].rearrange("(v k p) -> p v k", p=128, k=KC),
                                         allow_slow_non_contiguous=True), writes=["modc"])
        T.dma("sp", lambda: sp.dma_start(out=L["gB"][:], in_=mods[layer:layer + 1, 2 * D:3 * D].partition_broadcast(128)), writes=["gB"])
        T.dma("sp", lambda: sp.dma_start(out=L["lngB"][:], in_=I["mix_ln_g"][layer:layer + 1, :].partition_broadcast(128)), writes=["lngB"])
        T.dma("sp", lambda: sp.dma_start(out=L["lnbB"][:], in_=I["mix_ln_b"][layer:layer + 1, :].partition_broadcast(128)), writes=["lnbB"])
        T.dma("sp", lambda: sp.dma_start(out=L["lngc"][:], in_=I["mix_ln_g_c"][layer, :, :]), writes=["lngc"])
        T.dma("sp", lambda: sp.dma_start(out=L["lnbc"][:], in_=I["mix_ln_b_c"][layer, :, :]), writes=["lnbc"])
        T.dma("sp", lambda: sp.dma_start(out=L["rw"][:], in_=I["r_w"][layer, :, :].rearrange("(k p) e -> p k e", p=128)), writes=["rw"])
        T.dma("sp", lambda: sp.dma_start(out=L["rbrow"][:], in_=I["r_b"][layer:layer + 1, :]), writes=["rbrow"])
        T.op("dve", lambda: dve.tensor_scalar(out=L["gB"][:], in0=L["gB"][:], scalar1=1.0, scalar2=None, op0=ALU.add), reads=["gB"], writes=["gB"])
        T.op("dve", lambda: dve.tensor_scalar(out=L["sc1m"][:], in0=L["modc"][:, 1, :], scalar1=1.0, scalar2=None, op0=ALU.add),
             reads=["modc"], writes=["sc1m"])
        T.op("dve", lambda: dve.tensor_scalar(out=L["A2c"][:], in0=L["modc"][:, 4, :], scalar1=1.0, scalar2=None, op0=ALU.add),
             reads=["modc"], writes=["A2c"])
        T.op("dve", lambda: dve.tensor_tensor(out=L["B2c"][:], in0=L["A2c"][:], in1=L["lnbc"][:], op=ALU.mult), reads=["A2c", "lnbc"], writes=["B2c"])
        T.op("dve", lambda: dve.tensor_tensor(out=L["B2c"][:], in0=L["B2c"][:], in1=L["modc"][:, 3, :], op=ALU.add), reads=["B2c", "modc"], writes=["B2c"])
        T.op("dve", lambda: dve.tensor_tensor(out=L["A2c"][:], in0=L["A2c"][:], in1=L["lngc"][:], op=ALU.mult), reads=["A2c", "lngc"], writes=["A2c"])
        T.op("dve", lambda: dve.tensor_tensor(out=L["rwf"][:], in0=L["rw"][:], in1=L["A2c"][:].unsqueeze(2).to_broadcast([128, KC, 36]), op=ALU.mult),
             reads=["rw", "A2c"], writes=["rwf"])
        with ExitStack() as es:
            psb = self.ps(es, "psb", [128, 512])
            for k in range(KC):
                T.op("pe", lambda: pe.matmul(psb[0:1, 0:36], lhsT=L["B2c"][:, k:k + 1], rhs=L["rw"][:, k, :], start=(k == 0), stop=(k == KC - 1)),
                     reads=["B2c", "rw"], writes=["psb"], signal=(k == KC - 1))
            T.op("dve", lambda: dve.tensor_tensor(out=L["rbrow"][:], in0=L["rbrow"][:], in1=psb[0:1, 0:36], op=ALU.add),
                 reads=["psb", "rbrow"], writes=["rbrow"])
            T.barrier()

    def load_mixer_weights(self, layer):
        nc, T, I = self.nc, self.T, self.I
        pool = nc.gpsimd
        is_a = (layer % 2 == 0)
        j_ = layer // 2
        NT_IN = 16 if is_a else 24
        BIG = self.BIG
        w_in = BIG[:, 0:KC * NT_IN * 128].rearrange("p (k f) -> p k f", k=KC)
        o1 = KC * NT_IN * 128
        w_out = BIG[:, o1:o1 + KC * D].rearrange("p (k f) -> p k f", k=KC)
        win_d = I["a_w_in"] if is_a else I["b_w_in"]
        wout_d = I["a_w_out"] if is_a else I["b_w_out"]
        for k in range(KC):
            T.dma("pool", lambda: pool.dma_start(out=w_in[:, k, :], in_=win_d[j_, k * 128:(k + 1) * 128, :]), swrites=["w_in"])
        for k in range(KC):
            T.dma("pool", lambda: pool.dma_start(out=w_out[:, k, :], in_=wout_d[j_, k * 128:(k + 1) * 128, :]), swrites=["w_out"])
        self.mixw_prefetched = layer

    def phase_mixer(self, layer, src):
        nc, T, I, S, C, L, R = self.nc, self.T, self.I, self.S, self.C, self.L, self.R
        pe, act, dve, pool, sp = nc.tensor, nc.scalar, nc.vector, nc.gpsimd, nc.sync
        is_a = (layer % 2 == 0)
        j_ = layer // 2
        BIG = self.BIG
        NT_IN = 16 if is_a else 24
        NTAP = 31 if is_a else 3
        HIST = NTAP - 1
        w_in = BIG[:, 0:KC * NT_IN * 128].rearrange("p (k f) -> p k f", k=KC)
        o1 = KC * NT_IN * 128
        w_out = BIG[:, o1:o1 + KC * D].rearrange("p (k f) -> p k f", k=KC)
        o2 = o1 + KC * D
        diag = BIG[:, o2:o2 + KC * NTAP * 128].rearrange("p (j t m) -> p j t m", j=KC, t=NTAP)
        with ExitStack() as es:
            if getattr(self, "mixw_prefetched", None) != layer:
                self.load_mixer_weights(layer)
            wdw_d = I["a_w_dw_c"] if is_a else I["b_w_dw_c"]
            T.dma("sp", lambda: sp.dma_start(out=L["wdw_c"][:, :, 0:NTAP], in_=wdw_d[j_, :, :, :]), writes=["wdw_c"])
            if is_a:
                T.dma("sp", lambda: sp.dma_start(out=L["b_in_c"][:], in_=I["a_b_in_c"][j_, :, :]), writes=["b_in_c"])
                T.dma("sp", lambda: sp.dma_start(out=L["b_dw_c"][:], in_=I["a_b_dw_c"][j_, :, :]), writes=["b_dw_c"])
                T.dma("sp", lambda: sp.dma_start(out=L["aln_g_c"][:], in_=I["a_ln_g_c"][j_, :, :]), writes=["aln_g_c"])
                T.dma("sp", lambda: sp.dma_start(out=L["aln_b_c"][:], in_=I["a_ln_b_c"][j_, :, :]), writes=["aln_b_c"])
                T.dma("sp", lambda: sp.dma_start(out=L["b_out_f"][:], in_=I["a_b_out"][j_:j_ + 1, :]), writes=["b_out_f"])
                T.op("dve", lambda: dve.tensor_copy(out=L["b_out_b"][:], in_=L["b_out_f"][:]), reads=["b_out_f"], writes=["b_out_b"])
            for j in range(KC):
                T.op("pool", lambda: pool.tensor_tensor(
                    out=diag[:, j, :, :], in0=C["ident_b"][:].unsqueeze(1).to_broadcast([128, NTAP, 128]),
                    in1=L["wdw_c"][:, j, 0:NTAP].unsqueeze(2).to_broadcast([128, NTAP, 128]), op=ALU.mult),
                    reads=["ident_b", "wdw_c"], swrites=["diag"])

            import os
            MS = float(os.environ.get("MIX_STOP", "99"))
            if MS <= 1:
                T.barrier()
                return
            W = WA
            NTW = W // 128
            xt = self.sb(es, "xt", [128, 2, NTW, D], F32)
            hT = self.sb(es, "hT", [128, KC, W], BF16)
            ubuf = self.sb(es, "ubuf", [128, KC, HIST + W], BF16)
            sbf = self.sb(es, "sbf", [128, KC, W], BF16)
            tA = self.sb(es, "tA", [128, 2, W], F32)
            if is_a:
                vbf = self.sb(es, "vbf", [128, KC, W], BF16)
                sq = self.sb(es, "sq", [128, KC, W], BF16)
                mean = self.sb(es, "mean", [128, W], F32)
                rstd = self.sb(es, "rstd", [128, W], F32)
                nmr = self.sb(es, "nmr", [128, W], F32)
                tZ = self.sb(es, "tZ", [128, 2, W], F32)
            else:
                gbs = self.sb(es, "gbs", [128, 2, W], F32)
            r_t = self.sb(es, "r_t", [128, 1, D], F32)
            xn_t = self.sb(es, "xn_t", [128, 2, D], F32)
            xnb_t = self.sb(es, "xnb_t", [128, 1, D], BF16)
            xo_t = self.sb(es, "xo_t", [128, 1, D], F32)
            xnT = self.sb(es, "xnT", [128, KC, 128], F32)
            st6 = self.sb(es, "st6", [128, 4, 3], F32)
            mv = self.sb(es, "mv", [128, 2], F32)
            sm = self.sb(es, "sm", [128, 8], F32)
            lg = self.sb(es, "lg", [128, 36], F32)
            rt = self.sb(es, "rt", [128, 48], F32)
            el = self.sb(es, "el", [128, 2, NEXP], F32)
            pre = self.sb(es, "pre", [128, NEXP], F32)
            tmp32 = self.sb(es, "tmp32", [128, NEXP], F32)
            psT = self.ps(es, "psT", [128, 512])
            psAG = [self.ps(es, "psAG%d" % i, [128, 512]) for i in range(2)]
            psCv = [self.ps(es, "psCv%d" % i, [128, 512]) for i in range(2)]
            psS = self.ps(es, "psS", [128, 512])
            psY = [self.ps(es, "psY%d" % i, [128, 512]) for i in range(2)]

            T.op("dve", lambda: dve.memset(ubuf[:, :, 0:HIST], 0.0), swrites=[("ubuf", j) for j in range(KC)])
            T.op("dve", lambda: dve.memset(R["tot"][:], 0.0), writes=["tot"])

            stiles = [(0, 1)] + [(1 + NTW * s, NTW) for s in range((NTILE - 1) // NTW)]
            assert stiles[-1][0] + stiles[-1][1] == NTILE

            def load(si):
                t0, nt = stiles[si]
                b = si % 2
                T.dma("sp", lambda: sp.dma_start(out=xt[:, b, 0:nt, :], in_=src[t0 * 128:(t0 + nt) * 128, :].rearrange("(t p) d -> p t d", p=128)),
                      writes=[("xt", b)])

            def transposes(si_):
                t0_, nt_ = stiles[si_]
                b_ = si_ % 2
                Wc_ = nt_ * 128
                tbanks = [(psT, "b0"), (psY[0], ("psY", 0)), (psY[1], ("psY", 1))]
                for k in range(KC):
                    pst_, kst_ = tbanks[k % 3]
                    for jt in range(nt_):
                        T.op("pe", lambda: pe.transpose(pst_[:, jt * 128:(jt + 1) * 128], xt[:, b_, jt, k * 128:(k + 1) * 128], C["ident_f"][:]),
                             reads=[("xt", b_), "ident_f"], writes=[kst_], signal=(jt == nt_ - 1))
                    T.op("act", lambda: act.activation(out=hT[:, k, 0:Wc_], in_=pst_[:, 0:Wc_], func=AF.Identity,
                                                       scale=L["sc1m"][:, k:k + 1], bias=L["modc"][:, 0, k:k + 1]),
                         reads=["sc1m", "modc"], writes=[kst_, ("hT", k)])

            load(0)
            load(1)
            transposes(0)
            for si, (t0, nt) in enumerate(stiles):
                b = si % 2
                Wc = nt * 128
                if si >= 1 and si + 1 < len(stiles):
                    load(si + 1)
                hT_keys = [("hT", k) for k in range(KC)]
                if MS <= 2:
                    T.barrier()
                    return
                def inproj(j):
                    par = j % 2
                    kAG = ("bAG", par)
                    kC = ("bC", par)
                    pA = psAG[par][:, 0:Wc]
                    pG = psAG[par][:, 256:256 + Wc]
                    if is_a:
                        for k in range(KC):
                            T.op("pe", lambda: pe.matmul(pA, lhsT=w_in[:, k, j * 128:(j + 1) * 128], rhs=hT[:, k, 0:Wc],
                                                         start=(k == 0), stop=(k == KC - 1)),
                                 reads=hT_keys + ["w_in"], writes=[kAG], signal=False)
                        for k in range(KC):
                            T.op("pe", lambda: pe.matmul(pG, lhsT=w_in[:, k, D + j * 128:D + (j + 1) * 128], rhs=hT[:, k, 0:Wc],
                                                         start=(k == 0), stop=(k == KC - 1)),
                                 reads=hT_keys + ["w_in"], writes=[kAG], signal=(k == KC - 1))
                    else:
                        pV = psCv[par][:, 0:Wc]
                        for part, pst, kk_ in ((0, pA, kAG), (1, pG, kAG), (2, pV, kC)):
                            for k in range(KC):
                                T.op("pe", lambda: pe.matmul(pst, lhsT=w_in[:, k, part * D + j * 128:part * D + (j + 1) * 128],
                                                             rhs=hT[:, k, 0:Wc], start=(k == 0), stop=(k == KC - 1)),
                                     reads=hT_keys + ["w_in"], writes=[kk_], signal=(k == KC - 1 and part >= 1))

                def rest(j):
                    par = j % 2
                    kAG = ("bAG", par)
                    kC = ("bC", par)
                    pA = psAG[par][:, 0:Wc]
                    pG = psAG[par][:, 256:256 + Wc]
                    if is_a:
                        pCv = psCv[par][:, 0:Wc]
                        T.op("act", lambda: act.activation(out=tA[:, par, 0:Wc], in_=pG, func=AF.Sigmoid,
                                                           bias=L["b_in_c"][:, KC + j:KC + j + 1], scale=1.0),
                             reads=["b_in_c"], writes=[kAG, ("tA", par)])
                        T.op("dve", lambda: dve.scalar_tensor_tensor(out=ubuf[:, j, HIST:HIST + Wc], in0=pA,
                                                                     scalar=L["b_in_c"][:, j:j + 1], in1=tA[:, par, 0:Wc],
                                                                     op0=ALU.add, op1=ALU.mult),
                             reads=[("tA", par), "b_in_c"], writes=[kAG, ("ubuf", j)])
                    else:
                        pV = psCv[par][:, 0:Wc]
                        pCv = psCv[par][:, 256:256 + Wc]
                        T.op("act", lambda: act.copy(out=gbs[:, par, 0:Wc], in_=pA), writes=[kAG, ("gbs", par)])
                        T.op("act", lambda: act.copy(out=tA[:, par, 0:Wc], in_=pV), writes=[kC, ("tA", par)])
                        T.op("dve", lambda: dve.tensor_tensor(out=ubuf[:, j, HIST:HIST + Wc], in0=pG, in1=tA[:, par, 0:Wc], op=ALU.mult),
                             reads=[("tA", par)], writes=[kAG, ("ubuf", j)])
                    if si == 0:
                        T.op("dve", lambda: dve.tensor_scalar(out=ubuf[:, j, HIST:HIST + Wc], in0=ubuf[:, j, HIST:HIST + Wc],
                                                              scalar1=C["flag"][:, 0:1], scalar2=None, op0=ALU.mult),
                             reads=["flag"], writes=[("ubuf", j)])
                    return pCv, kC

                def conv_mm(j, pCv, kC):
                    for t in range(NTAP):
                        T.op("pe", lambda: pe.matmul(pCv, lhsT=diag[:, j, t, :], rhs=ubuf[:, j, t:t + Wc],
                                                     start=(t == 0), stop=(t == NTAP - 1)),
                             reads=[("ubuf", j), "diag"], writes=[kC], signal=(t == NTAP - 1))

                def conv_evac(j, pCv, kC):
                    par = j % 2
                    if is_a:
                        T.op("act", lambda: act.activation(out=vbf[:, j, 0:Wc], in_=pCv, func=AF.Identity,
                                                           bias=L["b_dw_c"][:, j:j + 1], scale=1.0),
                             reads=["b_dw_c"], writes=[kC, ("vbf", j)])
                        T.op("act", lambda: act.activation(out=sq[:, j, 0:Wc], in_=pCv, func=AF.Square,
                                                           bias=L["b_dw_c"][:, j:j + 1], scale=1.0),
                             reads=["b_dw_c"], writes=[kC, ("sq", j)])
                    else:
                        T.op("dve", lambda: dve.tensor_tensor(out=sbf[:, j, 0:Wc], in0=pCv, in1=gbs[:, par, 0:Wc], op=ALU.mult),
                             reads=[("gbs", par)], writes=[kC, ("sbf", j)])

                inproj(0)
                cur = rest(0)
                for j in range(KC):
                    pCv_, kC_ = cur
                    if j + 1 < KC:
                        inproj(j + 1)
                    self.pump(steps=1)
                    conv_mm(j, pCv_, kC_)
                    if j + 1 < KC:
                        cur = rest(j + 1)
                    conv_evac(j, pCv_, kC_)
                    self.pump(steps=1)
                if MS <= 3:
                    T.barrier()
                    return
                T.op("pool", lambda: pool.tensor_copy(out=ubuf[:, :, 0:HIST], in_=ubuf[:, :, Wc:Wc + HIST]),
                     reads=[("ubuf", j) for j in range(KC)], writes=[("ubuf", j) for j in range(KC)])
                if MS <= 3.2:
                    T.barrier()
                    return
                if is_a:
                    for j in range(KC):
                        T.op("pe", lambda: pe.matmul(psS[:, 0:Wc], lhsT=C["ones_b"][:], rhs=vbf[:, j, 0:Wc], start=(j == 0), stop=(j == KC - 1)),
                             reads=[("vbf", j), "ones_b"], writes=["b5"], signal=False)
                    for j in range(KC):
                        T.op("pe", lambda: pe.matmul(psS[:, 256:256 + Wc], lhsT=C["ones_b"][:], rhs=sq[:, j, 0:Wc], start=(j == 0), stop=(j == KC - 1)),
                             reads=[("sq", j), "ones_b"], writes=["b5"], signal=(j == KC - 1))
                    if MS <= 3.4:
                        T.barrier()
                        return
                    T.op("act", lambda: act.activation(out=mean[:, 0:Wc], in_=psS[:, 0:Wc], func=AF.Identity, scale=1.0 / D),
                         writes=["b5", "mean"])
                    if MS <= 3.42:
                        T.barrier()
                        return
                    T.op("act", lambda: act.activation(out=rstd[:, 0:Wc], in_=psS[:, 256:256 + Wc], func=AF.Identity, scale=1.0 / D),
                         writes=["b5", "rstd"])
                    if MS <= 3.44:
                        T.barrier()
                        return
                    if MS <= 3.45:
                        T.barrier()
                        return
                    T.op("dve", lambda: dve.tensor_tensor(out=nmr[:, 0:Wc], in0=mean[:, 0:Wc], in1=mean[:, 0:Wc], op=ALU.mult),
                         reads=["mean"], writes=["nmr"])
                    if MS <= 3.5:
                        T.barrier()
                        return
                    T.op("dve", lambda: dve.tensor_tensor(out=rstd[:, 0:Wc], in0=rstd[:, 0:Wc], in1=nmr[:, 0:Wc], op=ALU.subtract),
                         reads=["rstd", "nmr"], writes=["rstd"])
                    if MS <= 3.6:
                        T.barrier()
                        return
                    T.op("act", lambda: act.activation(out=rstd[:, 0:Wc], in_=rstd[:, 0:Wc], func=AF.Sqrt, bias=C["eps"][:, 0:1], scale=1.0),
                         reads=["rstd", "eps"], writes=["rstd"])
                    T.op("dve", lambda: dve.reciprocal(out=rstd[:, 0:Wc], in_=rstd[:, 0:Wc]), reads=["rstd"], writes=["rstd"])
                    T.op("dve", lambda: dve.scalar_tensor_tensor(out=nmr[:, 0:Wc], in0=mean[:, 0:Wc], scalar=C["kc"][:, 1:2], in1=rstd[:, 0:Wc],
                                                                 op0=ALU.mult, op1=ALU.mult),
                         reads=["mean", "rstd"], writes=["nmr"])
                    if MS <= 3.8:
                        T.barrier()
                        return
                    for j in range(KC):
                        T.op("dve", lambda: dve.tensor_tensor(out=tZ[:, j % 2, 0:Wc], in0=vbf[:, j, 0:Wc], in1=rstd[:, 0:Wc], op=ALU.mult),
                             reads=[("vbf", j), "rstd"], writes=[("tZ", j % 2)])
                        T.op("dve", lambda: dve.tensor_tensor(out=tZ[:, j % 2, 0:Wc], in0=tZ[:, j % 2, 0:Wc], in1=nmr[:, 0:Wc], op=ALU.add),
                             reads=[("tZ", j % 2), "nmr"], writes=[("tZ", j % 2)])
                        T.op("act", lambda: act.activation(out=sbf[:, j, 0:Wc], in_=tZ[:, j % 2, 0:Wc], func=AF.Silu,
                                                           scale=L["aln_g_c"][:, j:j + 1], bias=L["aln_b_c"][:, j:j + 1]),
                             reads=[("tZ", j % 2), "aln_g_c", "aln_b_c"], writes=[("sbf", j)])
                if MS <= 4:
                    T.barrier()
                    return
                for jt in range(nt):
                    tile = t0 + jt
                    tb = tile % 2
                    for hf in range(2):
                        for j in range(KC):
                            last = (j == KC - 1) and not is_a
                            T.op("pe", lambda: pe.matmul(psY[hf][:, :], lhsT=sbf[:, j, jt * 128:(jt + 1) * 128], rhs=w_out[:, j, hf * 512:(hf + 1) * 512],
                                                         start=(j == 0), stop=last),
                                 reads=[("sbf", j), "w_out"], writes=[("psY", hf)], signal=last)
                        if is_a:
                            T.op("pe", lambda: pe.matmul(psY[hf][:, :], lhsT=L["ones_row_b"][0:1, :], rhs=L["b_out_b"][0:1, hf * 512:(hf + 1) * 512],
                                                         start=False, stop=True),
                                 reads=["ones_row_b", "b_out_b"], writes=[("psY", hf)])
                    self.pump(steps=0, upto=tile - 2)
                    hook = None
                    if jt == nt - 1 and si + 1 < len(stiles):
                        hook = (lambda si1=si + 1: transposes(si1))
                    self.epilogue(tile, tb, psY, xt[:, b, jt, :], ("xt", b), r_t, xn_t, xnb_t, xo_t, st6, mv, sm, S["xres"], True, hook=hook)
                    self.pending.append((tile, self.router_gen(tile, tb, xn_t, xnT, psT, psS, lg, rt, el, pre, tmp32)))
            self.pump(drain=True)
            T.barrier()

    def epilogue(self, tile, tb, psY, x_ap, x_key, r_t, xn_t, xnb_t, xo_t, st6, mv, sm, dst, want_xnb, y_sb=None, dst_row0=None, aff="pool", hook=None):
        nc, T, C, L, S = self.nc, self.T, self.C, self.L, self.S
        pe, act, dve, pool, sp = nc.tensor, nc.scalar, nc.vector, nc.gpsimd, nc.sync
        rb = tb % r_t.shape[1]
        ob = tb % xo_t.shape[1]
        r = r_t[:, rb, :]
        if y_sb is None:
            for hf in range(2):
                T.op("dve", lambda: dve.tensor_tensor(out=r[:, hf * 512:(hf + 1) * 512], in0=psY[hf][:, :], in1=L["gB"][:, hf * 512:(hf + 1) * 512], op=ALU.mult),
                     reads=["gB"], writes=[("psY", hf)], swrites=[("r", rb)])
        else:
            T.op("dve", lambda: dve.tensor_tensor(out=r, in0=y_sb[0], in1=L["gB"][:], op=ALU.mult), reads=[y_sb[1], "gB"], swrites=[("r", rb)])
        T.op("dve", lambda: dve.scalar_tensor_tensor(out=r, in0=x_ap, scalar=C["kc"][:, 0:1], in1=r, op0=ALU.mult, op1=ALU.add),
             reads=[x_key, ("r", rb)], writes=[("r", rb)])
        T.op("dve", lambda: dve.bn_stats(out=st6[:, 0:2, :], in_=r[:, 0:512]), reads=[("r", rb)], swrites=["st6"])
        T.op("dve", lambda: dve.bn_stats(out=st6[:, 2:4, :], in_=r[:, 512:1024]), reads=[("r", rb)], swrites=["st6"])
        T.op("dve", lambda: dve.bn_aggr(out=mv[:], in_=st6[:]), reads=["st6"], writes=["mv"])
        if hook is not None:
            hook()
        T.op("act", lambda: act.activation(out=sm[:, 0:1], in_=mv[:, 1:2], func=AF.Sqrt, bias=C["eps"][:, 0:1], scale=1.0), reads=["mv", "eps"], writes=["sm0"])
        T.op("dve", lambda: dve.reciprocal(out=sm[:, 1:2], in_=sm[:, 0:1]), reads=["sm0"], writes=["sm1"])
        T.op("dve", lambda: dve.scalar_tensor_tensor(out=sm[:, 2:3], in0=mv[:, 0:1], scalar=C["kc"][:, 1:2], in1=sm[:, 1:2], op0=ALU.mult, op1=ALU.mult),
             reads=["mv", "sm1"], writes=["sm2"])
        xn = xn_t[:, tb, :]
        if aff == "pool":
            T.op("pool", lambda: pool.tensor_scalar(out=xn, in0=r, scalar1=sm[:, 1:2], scalar2=sm[:, 2:3], op0=ALU.mult, op1=ALU.add),
                 reads=[("r", rb), "sm1", "sm2"], writes=[("xn", tb)])
        else:
            T.op("act", lambda: act.activation(out=xn, in_=r, func=AF.Identity, scale=sm[:, 1:2], bias=sm[:, 2:3]),
                 reads=[("r", rb), "sm1", "sm2"], writes=[("xn", tb)])
        if want_xnb:
            nb = tb % xnb_t.shape[1]
            T.op("pool", lambda: pool.tensor_copy(out=xnb_t[:, nb, :], in_=xn), reads=[("xn", tb)], writes=[("xnb", nb)])
            T.dma("sp", lambda: sp.dma_start(out=S["xnb"][tile * 128:(tile + 1) * 128, :], in_=xnb_t[:, nb, :]),
                  reads=[("xnb", nb)], swrites=["xnb_d"])
        xo = xo_t[:, ob, :]
        an = "dve" if aff == "mix" else "pool"
        ae = self.T.eng[an]
        T.op(an, lambda: ae.tensor_tensor(out=xo, in0=xn, in1=L["lngB"][:], op=ALU.mult), reads=[("xn", tb), "lngB"], writes=[("xo", ob)])
        T.op(an, lambda: ae.tensor_tensor(out=xo, in0=xo, in1=L["lnbB"][:], op=ALU.add), reads=[("xo", ob), "lnbB"], writes=[("xo", ob)])
        row0 = tile * 128 if dst_row0 is None else dst_row0
        T.dma("sp", lambda: sp.dma_start(out=dst[row0:row0 + 128, :], in_=xo), reads=[("xo", ob)], swrites=["xres_d"])

    def router_gen(self, tile, tb, xn_t, xnT, psT, psS, lg, rt, el, pre, tmp32):
        nc, T, C, L, R = self.nc, self.T, self.C, self.L, self.R
        pe, act, dve, pool, sp = nc.tensor, nc.scalar, nc.vector, nc.gpsimd, nc.sync
        xn = xn_t[:, tb, :]
        for rnd in range(2):
            for kk in range(4):
                k = rnd * 4 + kk
                T.op("pe", lambda: pe.transpose(psT[:, kk * 128:(kk + 1) * 128], xn[:, k * 128:(k + 1) * 128], C["ident_f"][:]),
                     reads=[("xn", tb), "ident_f"], writes=["b0"], signal=(kk == 3))
            T.op("act", lambda: act.copy(out=xnT[:, rnd * 4:(rnd + 1) * 4, :], in_=psT[:, :].rearrange("p (k m) -> p k m", k=4)),
                 writes=["b0"], swrites=["xnT"])
            yield
        psL = psS[:, 0:36]
        psP = psS[:, 64:96]
        psQ = psS[:, 128:160]
        for k in range(KC):
            T.op("pe", lambda: pe.matmul(psL, lhsT=xnT[:, k, :], rhs=L["rwf"][:, k, :], start=(k == 0), stop=False),
                 reads=["xnT", "rwf"], writes=["b5"], signal=False)
        T.op("pe", lambda: pe.matmul(psL, lhsT=L["ones_row_f"][0:1, :], rhs=L["rbrow"][0:1, :], start=False, stop=True),
             reads=["ones_row_f", "rbrow"], writes=["b5"])
        T.op("dve", lambda: dve.tensor_copy(out=lg[:], in_=psL), writes=["b5", "lg"])
        yield
        T.op("dve", lambda: dve.tensor_reduce(out=rt[:, 0:1], in_=lg[:, 0:4], axis=AX.X, op=ALU.max), reads=["lg"], writes=["rt0"])
        T.op("dve", lambda: dve.tensor_scalar(out=rt[:, 1:2], in0=rt[:, 0:1], scalar1=-1.0, scalar2=None, op0=ALU.mult), reads=["rt0"], writes=["rt1"])
        T.op("act", lambda: act.activation(out=rt[:, 16:20], in_=lg[:, 0:4], func=AF.Exp, bias=rt[:, 1:2], scale=1.0), reads=["lg", "rt1"], writes=["rt16"])
        T.op("dve", lambda: dve.tensor_reduce(out=rt[:, 2:3], in_=rt[:, 16:20], axis=AX.X, op=ALU.add), reads=["rt16"], writes=["rt2"])
        T.op("dve", lambda: dve.reciprocal(out=rt[:, 3:4], in_=rt[:, 2:3]), reads=["rt2"], writes=["rt3"])
        T.op("dve", lambda: dve.tensor_scalar(out=rt[:, 12:16], in0=lg[:, 0:4], scalar1=rt[:, 0:1], scalar2=None, op0=ALU.is_ge), reads=["lg", "rt0"], writes=["rt12"])
        T.op("dve", lambda: dve.tensor_scalar(out=rt[:, 12:16], in0=rt[:, 12:16], scalar1=-1.0, scalar2=1e30, op0=ALU.add, op1=ALU.mult), reads=["rt12"], writes=["rt12"])
        T.op("dve", lambda: dve.tensor_tensor(out=el[:, 0, :].rearrange("p (g e) -> p g e", g=4), in0=lg[:, 4:36].rearrange("p (g e) -> p g e", g=4),
                                              in1=rt[:, 12:16].unsqueeze(2).to_broadcast([128, 4, 8]), op=ALU.add), reads=["lg", "rt12"], writes=["el0"])
        yield
        T.op("dve", lambda: dve.tensor_reduce(out=rt[:, 4:5], in_=el[:, 0, :], axis=AX.X, op=ALU.max), reads=["el0"], writes=["rt4"])
        oh1 = R["oh1"][:, tile, :]
        oh2 = R["oh2"][:, tile, :]
        T.op("dve", lambda: dve.tensor_scalar(out=oh1, in0=el[:, 0, :], scalar1=rt[:, 4:5], scalar2=None, op0=ALU.is_ge), reads=["el0", "rt4"], writes=["oh1"])
        T.op("dve", lambda: dve.scalar_tensor_tensor(out=el[:, 1, :], in0=oh1, scalar=C["kc"][:, 3:4], in1=el[:, 0, :], op0=ALU.mult, op1=ALU.add),
             reads=["oh1", "el0"], writes=["el1"])
        T.op("dve", lambda: dve.tensor_reduce(out=rt[:, 5:6], in_=el[:, 1, :], axis=AX.X, op=ALU.max), reads=["el1"], writes=["rt5"])
        T.op("dve", lambda: dve.tensor_scalar(out=oh2, in0=el[:, 1, :], scalar1=rt[:, 5:6], scalar2=None, op0=ALU.is_ge), reads=["el1", "rt5"], writes=["oh2"])
        T.op("dve", lambda: dve.tensor_tensor(out=rt[:, 6:7], in0=rt[:, 4:5], in1=rt[:, 5:6], op=ALU.subtract), reads=["rt4", "rt5"], writes=["rt6"])
        T.op("act", lambda: act.activation(out=rt[:, 7:8], in_=rt[:, 6:7], func=AF.Sigmoid), reads=["rt6"], writes=["rt7"])
        w1 = R["w1"][:, tile:tile + 1]
        w2 = R["w2"][:, tile:tile + 1]
        T.op("dve", lambda: dve.tensor_tensor(out=w1, in0=rt[:, 7:8], in1=rt[:, 3:4], op=ALU.mult), reads=["rt7", "rt3"], writes=["w1"])
        T.op("dve", lambda: dve.tensor_tensor(out=w2, in0=rt[:, 3:4], in1=w1, op=ALU.subtract), reads=["rt3", "w1"], writes=["w2"])
        T.op("dve", lambda: dve.tensor_tensor(out=tmp32[:], in0=oh1, in1=oh2, op=ALU.add), reads=["oh1", "oh2"], writes=["sel"])
        yield
        T.op("pe", lambda: pe.matmul(psP, lhsT=C["tri_f"][:], rhs=tmp32[:], start=True, stop=True), reads=["sel", "tri_f"], writes=["b5"], signal=False)
        T.op("pe", lambda: pe.matmul(psQ, lhsT=C["ones_f"][:], rhs=tmp32[:], start=True, stop=True), reads=["sel", "ones_f"], writes=["b5"])
        T.op("dve", lambda: dve.tensor_tensor(out=pre[:], in0=psP, in1=R["tot"][:], op=ALU.add), reads=["tot"], writes=["b5", "pre"])
        T.op("dve", lambda: dve.tensor_tensor(out=R["tot"][:], in0=psQ, in1=R["tot"][:], op=ALU.add), reads=["pre"], writes=["b5", "tot"])
        T.op("dve", lambda: dve.tensor_tensor(out=tmp32[:], in0=pre[:], in1=oh1, op=ALU.mult), reads=["pre", "oh1"], writes=["sel"])
        T.op("dve", lambda: dve.tensor_reduce(out=R["pos1"][:, tile:tile + 1], in_=tmp32[:], axis=AX.X, op=ALU.add), reads=["sel"], writes=["pos1"])
        T.op("dve", lambda: dve.tensor_tensor(out=tmp32[:], in0=pre[:], in1=oh2, op=ALU.mult), reads=["pre", "oh2", "pos1"], writes=["sel"])
        T.op("dve", lambda: dve.tensor_reduce(out=R["pos2"][:, tile:tile + 1], in_=tmp32[:], axis=AX.X, op=ALU.add), reads=["sel"], writes=["pos2"])

    def phase_route_finish(self, layer):
        nc, T, C, L, R, S = self.nc, self.T, self.C, self.L, self.R, self.S
        pe, act, dve, pool, sp = nc.tensor, nc.scalar, nc.vector, nc.gpsimd, nc.sync
        with ExitStack() as es:
            ci = self.sb(es, "ci", [128, NEXP], I32)
            padf = self.sb(es, "padf", [128, NEXP], F32)
            sc = self.sb(es, "sc", [128, 2, NEXP], F32)
            pend = self.sb(es, "pend", [128, NEXP], F32)
            pstart = self.sb(es, "pstart", [128, NEXP], F32)
            big = self.sb(es, "big", [128, NTILE, NEXP], F32)
            dsum = self.sb(es, "dsum", [128, max(NTILE, NBIG)], F32)
            cmp3 = self.sb(es, "cmp3", [128, NBIG, NEXP], F32)
            berow = self.sb(es, "berow", [128, NBIG], F32)
            T.op("dve", lambda: dve.tensor_scalar(out=padf[:], in0=R["tot"][:], scalar1=float(BS - 1) - (BS / 2.0 - 0.5), scalar2=1.0 / BS,
                                                  op0=ALU.add, op1=ALU.mult), writes=["padf"])
            T.op("dve", lambda: dve.tensor_copy(out=ci[:], in_=padf[:]), reads=["padf"], writes=["ci"])
            T.op("dve", lambda: dve.tensor_copy(out=padf[:], in_=ci[:]), reads=["ci"], writes=["padf"])
            T.op("dve", lambda: dve.tensor_scalar(out=padf[:], in0=padf[:], scalar1=float(BS), scalar2=None, op0=ALU.mult), reads=["padf"], writes=["padf"])
            T.op("dve", lambda: dve.tensor_copy(out=sc[:, 0, :], in_=padf[:]), reads=["padf"], writes=[("sc", 0)])
            cur = 0
            for sh in (1, 2, 4, 8, 16):
                nxt = 1 - cur
                T.op("dve", lambda: dve.tensor_copy(out=sc[:, nxt, 0:sh], in_=sc[:, cur, 0:sh]), reads=[("sc", cur)], swrites=[("sc", nxt)])
                T.op("dve", lambda: dve.tensor_tensor(out=sc[:, nxt, sh:NEXP], in0=sc[:, cur, sh:NEXP], in1=sc[:, cur, 0:NEXP - sh], op=ALU.add),
                     reads=[("sc", cur)], swrites=[("sc", nxt)])
                cur = nxt
            T.op("dve", lambda: dve.tensor_copy(out=pend[:], in_=sc[:, cur, :]), reads=[("sc", cur)], writes=["pend"])
            T.op("dve", lambda: dve.tensor_tensor(out=pstart[:], in0=pend[:], in1=padf[:], op=ALU.subtract), reads=["pend", "padf"], writes=["pstart"])
            for nm, ohk, posk, destk in (("a", "oh1", "pos1", "dest1"), ("b", "oh2", "pos2", "dest2")):
                T.op("dve", lambda: dve.tensor_tensor(out=big[:], in0=R[ohk][:], in1=pstart[:].unsqueeze(1).to_broadcast([128, NTILE, NEXP]), op=ALU.mult),
                     reads=["pstart"], writes=["big"])
                T.op("dve", lambda: dve.tensor_reduce(out=dsum[:, 0:NTILE], in_=big[:], axis=AX.X, op=ALU.add), reads=["big"], writes=["dsum"])
                T.op("dve", lambda: dve.tensor_tensor(out=dsum[:, 0:NTILE], in0=dsum[:, 0:NTILE], in1=R[posk][:], op=ALU.add), reads=["dsum"], writes=["dsum"])
                T.op("dve", lambda: dve.tensor_copy(out=R[destk][:], in_=dsum[:, 0:NTILE]), reads=["dsum"], writes=[destk])
            T.op("dve", lambda: dve.tensor_tensor(out=cmp3[:], in0=pend[:].unsqueeze(1).to_broadcast([128, NBIG, NEXP]),
                                                  in1=C["blk_start"][:].unsqueeze(2).to_broadcast([128, NBIG, NEXP]), op=ALU.is_le),
                 reads=["pend", "blk_start"], writes=["cmp3"])
            T.op("dve", lambda: dve.tensor_reduce(out=berow[:], in_=cmp3[:], axis=AX.X, op=ALU.add), reads=["cmp3"], writes=["berow"])
            T.op("dve", lambda: dve.tensor_scalar(out=berow[:], in0=berow[:], scalar1=31.0, scalar2=128.0, op0=ALU.min, op1=ALU.mult), reads=["berow"], writes=["berow"])
            T.op("dve", lambda: dve.tensor_scalar(out=berow[:], in0=berow[:], scalar1=C["iota_p"][:, 0:1], scalar2=None, op0=ALU.add), reads=["berow", "iota_p"], writes=["berow"])
            T.op("dve", lambda: dve.tensor_copy(out=R["widx"][:], in_=berow[:]), reads=["berow"], writes=["widx"])
            if "dbg_route" in self.dbg:
                dr = self.dbg["dbg_route"]
                T.dma("sp", lambda: sp.dma_start(out=dr[:, 0:NTILE], in_=R["dest1"][:]), reads=["dest1"])
                T.dma("sp", lambda: sp.dma_start(out=dr[:, NTILE:2 * NTILE], in_=R["dest2"][:]), reads=["dest2"])
                T.dma("sp", lambda: sp.dma_start(out=dr[:, 2 * NTILE:2 * NTILE + NBIG], in_=R["widx"][:]), reads=["widx"])
                dw = self.dbg["dbg_w"]
                T.dma("sp", lambda: sp.dma_start(out=dw[:, 0:NTILE], in_=R["w1"][:]))
                T.dma("sp", lambda: sp.dma_start(out=dw[:, NTILE:2 * NTILE], in_=R["w2"][:]))
            T.barrier()

    def phase_scatter(self, layer):
        nc, T, C, L, R, S = self.nc, self.T, self.C, self.L, self.R, self.S
        pe, act, dve, pool, sp = nc.tensor, nc.scalar, nc.vector, nc.gpsimd, nc.sync
        with ExitStack() as es:
            xbl = [self.sb(es, "xb_s%d" % i, [128, D], BF16) for i in range(3)]
            for tile in range(NTILE):
                b = tile % 3
                T.dma("sp", lambda: sp.dma_start(out=xbl[b][:, :], in_=S["xnb"][tile * 128:(tile + 1) * 128, :]), writes=[("xb", b)])
                for dk in ("dest1", "dest2"):
                    T.dma("pool", lambda: pool.indirect_dma_start(
                        out=S["xs"][:, :], out_offset=bass.IndirectOffsetOnAxis(ap=R[dk][:, tile:tile + 1], axis=0),
                        in_=xbl[b][:, :], in_offset=None),
                        reads=[("xb", b)], swrites=["xs_d"])
            T.barrier()

    def phase_experts(self, layer):
        nc, T, I, C, L, R, S = self.nc, self.T, self.I, self.C, self.L, self.R, self.S
        pe, act, dve, pool, sp = nc.tensor, nc.scalar, nc.vector, nc.gpsimd, nc.sync
        ESZ = 3 * 4096
        NWB = 4
        wbuf = self.BIG[:, 0:NWB * ESZ].rearrange("p (s f) -> p s f", s=NWB)
        with ExitStack() as es:
            xin = self.sb(es, "xin_e", [128, 3, D], BF16)
            xT = self.sb(es, "xT_e", [128, 2, KC, 128], BF16)
            sg = self.sb(es, "sg_e", [128, 2, DE], F32)
            hid = self.sb(es, "hid_e", [128, 2, DE], BF16)
            hidT = self.sb(es, "hidT_e", [128, 2, 4, 128], BF16)
            yo = self.sb(es, "yo_e", [128, 2, D], F32)
            psX = [self.ps(es, "psX%d" % i, [128, D], BF16) for i in range(2)]
            psGt = self.ps(es, "psGt", [128, 512])
            psUp = self.ps(es, "psUp", [128, 512])
            psH = [self.ps(es, "psH%d" % i, [128, 1024], BF16) for i in range(2)]
            psYe = [self.ps(es, "psYe%d" % i, [128, 512]) for i in range(2)]

            def wload(B):
                wb = B % NWB
                for mi, nm in enumerate(("e_w_gate", "e_w_up", "e_w_down")):
                    for hh in range(2):
                        T.dma("pool", lambda: pool.indirect_dma_start(
                            out=wbuf[:, wb, mi * 4096 + hh * 2048:mi * 4096 + (hh + 1) * 2048], out_offset=None,
                            in_=I["%s_%d_%d" % (nm, layer, hh)][:, :],
                            in_offset=bass.IndirectOffsetOnAxis(ap=R["widx"][:, B:B + 1], axis=0),
                            ),
                            reads=["widx"], swrites=[("wbuf", wb)])

            NSB = NBIG * NSUB

            def load(sbk):
                b3 = sbk % 3
                T.dma("sp", lambda: sp.dma_start(out=xin[:, b3, :], in_=S["xs"][sbk * 128:(sbk + 1) * 128, :]), writes=[("xin", b3)])

            def s1(bk):
                b2, b3 = bk % 2, bk % 3
                for k in range(KC):
                    hb, kk = k // 4, k % 4
                    T.op("pe", lambda: pe.transpose(psX[hb][:, kk * 128:(kk + 1) * 128], xin[:, b3, k * 128:(k + 1) * 128], C["ident_b"][:]),
                         reads=[("xin", b3), "ident_b"], writes=[("psX", hb)], signal=(kk == 3))
                for kk in range(4):
                    k = kk
                    T.op("act", lambda: act.activation(out=xT[:, b2, k, :], in_=psX[0][:, kk * 128:(kk + 1) * 128], func=AF.Identity,
                                                       scale=L["A2c"][:, k:k + 1], bias=L["B2c"][:, k:k + 1]),
                         reads=["A2c", "B2c"], writes=[("psX", 0)], swrites=[("xT", b2)])
                    k2 = 4 + kk
                    T.op("dve", lambda: dve.tensor_scalar(out=xT[:, b2, k2, :], in0=psX[1][:, kk * 128:(kk + 1) * 128],
                                                          scalar1=L["A2c"][:, k2:k2 + 1], scalar2=L["B2c"][:, k2:k2 + 1], op0=ALU.mult, op1=ALU.add),
                         reads=["A2c", "B2c"], writes=[("psX", 1)], swrites=[("xT", b2)])
                if bk + 3 < NSB:
                    load(bk + 3)

            def s2(bk):
                b2 = bk % 2
                wb = (bk // NSUB) % NWB
                w = wbuf[:, wb, :]
                for nm, pst, off in (("g", psGt, 0), ("u", psUp, 4096)):
                    for k in range(KC):
                        T.op("pe", lambda: pe.matmul(pst[:, :], lhsT=xT[:, b2, k, :], rhs=w[:, off + k * 512:off + (k + 1) * 512],
                                                     start=(k == 0), stop=(k == KC - 1)),
                             reads=[("xT", b2), ("wbuf", wb)], writes=["ps_" + nm], signal=(k == KC - 1))
                T.op("act", lambda: act.activation(out=sg[:, b2, :], in_=psGt[:, :], func=AF.Silu), writes=["ps_g", ("sg", b2)])
                T.op("dve", lambda: dve.tensor_tensor(out=hid[:, b2, :], in0=psUp[:, :], in1=sg[:, b2, :], op=ALU.mult),
                     reads=[("sg", b2)], writes=["ps_u", ("hid", b2)])

            def s3(bk):
                b2 = bk % 2
                for jc in range(4):
                    T.op("pe", lambda: pe.transpose(psH[b2][:, jc * 128:(jc + 1) * 128], hid[:, b2, jc * 128:(jc + 1) * 128], C["ident_b"][:]),
                         reads=[("hid", b2), "ident_b"], writes=[("psH", b2)], signal=(jc == 3))
                T.op("dve", lambda: dve.tensor_copy(out=hidT[:, b2, :, :], in_=psH[b2][:, 0:512].rearrange("p (j m) -> p j m", j=4)),
                     writes=[("psH", b2), ("hidT", b2)])

            def s4(bk):
                b2 = bk % 2
                wb = (bk // NSUB) % NWB
                w = wbuf[:, wb, :]
                for hf in range(2):
                    for jc in range(4):
                        T.op("pe", lambda: pe.matmul(psYe[hf][:, :], lhsT=hidT[:, b2, jc, :],
                                                     rhs=w[:, 8192 + jc * 1024 + hf * 512:8192 + jc * 1024 + (hf + 1) * 512],
                                                     start=(jc == 0), stop=(jc == 3)),
                             reads=[("hidT", b2), ("wbuf", wb)], writes=[("psYe", hf)], signal=(jc == 3))
                T.op("act", lambda: act.copy(out=yo[:, b2, 0:512], in_=psYe[0][:, :]), writes=[("psYe", 0)], swrites=[("yo", b2)])
                T.op("dve", lambda: dve.tensor_copy(out=yo[:, b2, 512:1024], in_=psYe[1][:, :]), writes=[("psYe", 1)], swrites=[("yo", b2)])
                T.dma("sp", lambda: sp.dma_start(out=S["yb"][bk * 128:(bk + 1) * 128, :], in_=yo[:, b2, :]), reads=[("yo", b2)], swrites=["yb_d"])
                if bk % NSUB == NSUB - 1:
                    Bn = bk // NSUB + NWB
                    if Bn < NBIG:
                        wload(Bn)

            for B0 in range(NWB):
                wload(B0)
            load(0)
            load(1)
            load(2)
            s1(0)
            s2(0)
            if NSB > 1:
                s1(1)
            for bk in range(NSB):
                s3(bk)
                if bk + 1 < NSB:
                    s2(bk + 1)
                if bk + 2 < NSB:
                    s1(bk + 2)
                s4(bk)
            T.barrier()

    def phase_combine(self, layer, is_last):
        nc, T, I, C, L, R, S = self.nc, self.T, self.I, self.C, self.L, self.R, self.S
        pe, act, dve, pool, sp = nc.tensor, nc.scalar, nc.vector, nc.gpsimd, nc.sync
        mods = S["mods"]
        T.dma("sp", lambda: sp.dma_start(out=L["gB"][:], in_=mods[layer:layer + 1, 5 * D:6 * D].partition_broadcast(128)), writes=["gB"])
        T.dma("sp", lambda: sp.dma_start(out=L["lngB"][:], in_=I["ffn_ln_g"][layer:layer + 1, :].partition_broadcast(128)), writes=["lngB"])
        T.dma("sp", lambda: sp.dma_start(out=L["lnbB"][:], in_=I["ffn_ln_b"][layer:layer + 1, :].partition_broadcast(128)), writes=["lnbB"])
        T.op("dve", lambda: dve.tensor_scalar(out=L["gB"][:], in0=L["gB"][:], scalar1=1.0, scalar2=None, op0=ALU.add), reads=["gB"], writes=["gB"])
        if not is_last and (layer + 1) in self.layers:
            self.load_mixer_weights(layer + 1)
        with ExitStack() as es:
            y1l = [self.sb(es, "y1_c%d" % i, [128, D], F32) for i in range(2)]
            y2l = [self.sb(es, "y2_c%d" % i, [128, D], F32) for i in range(2)]
            xm = self.sb(es, "xm_c", [128, 2, D], F32)
            r_t = self.sb(es, "r_c", [128, 2, D], F32)
            xn_t = self.sb(es, "xn_c", [128, 2, D], F32)
            xo_t = self.sb(es, "xo_c", [128, 2, D], F32)
            st6 = self.sb(es, "st6_c", [128, 4, 3], F32)
            mv = self.sb(es, "mv_c", [128, 2], F32)
            sm = self.sb(es, "sm_c", [128, 8], F32)
            final = is_last and self.last
            mg = None
            if not is_last and (layer + 1) in self.layers:
                mg = self.mods_gen(layer + 1, es, pw=512, nbuf=1)
            for tile in range(NTILE):
                if mg is not None and tile % 2 == 1:
                    try:
                        next(mg)
                    except StopIteration:
                        mg = None
                if final and tile == 0:
                    continue
                tb = tile % 2
                T.dma("sp", lambda: sp.dma_start(out=xm[:, tb, :], in_=S["xres"][tile * 128:(tile + 1) * 128, :]), writes=[("xm", tb)])
                y1 = y1l[tb]
                y2 = y2l[tb]
                for yk, dk in ((y1, "dest1"), (y2, "dest2")):
                    T.dma("pool", lambda: pool.indirect_dma_start(
                        out=yk[:, :], out_offset=None, in_=S["yb"][:, :],
                        in_offset=bass.IndirectOffsetOnAxis(ap=R[dk][:, tile:tile + 1], axis=0),
                        ), writes=[(dk + "y", tb)])
                T.op("act", lambda: act.activation(out=y1[:, :], in_=y1[:, :], func=AF.Identity, scale=R["w1"][:, tile:tile + 1]),
                     reads=[("dest1y", tb)], writes=[("dest1y", tb)])
                T.op("dve", lambda: dve.scalar_tensor_tensor(out=y1[:, :], in0=y2[:, :], scalar=R["w2"][:, tile:tile + 1], in1=y1[:, :],
                                                              op0=ALU.mult, op1=ALU.add),
                     reads=[("dest1y", tb), ("dest2y", tb)], writes=[("dest1y", tb)])
                if final:
                    self.epilogue(tile, tb, None, xm[:, tb, :], ("xm", tb), r_t, xn_t, None, xo_t, st6, mv, sm, self.out, False,
                                  y_sb=(y1[:, :], ("dest1y", tb)), dst_row0=(tile - 1) * 128, aff="mix")
                else:
                    dst = S["xres"] if not is_last else self.out
                    self.epilogue(tile, tb, None, xm[:, tb, :], ("xm", tb), r_t, xn_t, None, xo_t, st6, mv, sm, dst, False,
                                  y_sb=(y1[:, :], ("dest1y", tb)), aff="mix")
            if mg is not None:
                for _ in mg:
                    pass
            T.barrier()


def _core_inputs(inputs, core):
    b, half = core // 2, core % 2
    x = inputs["x"]
    xin = np.zeros((NTOK, D), np.float32)
    if half == 1:
        xin[:] = x[b, NOWN - HALO:2 * NOWN]
    else:
        xin[HALO:] = x[b, 0:NOWN]
    return {
        "x_in": xin,
        "flag": np.full((128, 1), float(half), np.float32),
        "c_col": np.ascontiguousarray(inputs["c"][b].reshape(KC, 128).T),
    }


def _shared_inputs(inputs):
    f = lambda a: np.ascontiguousarray(np.asarray(a, dtype=np.float32))
    col = lambda a: np.ascontiguousarray(a.reshape(a.shape[0], -1, 128).transpose(0, 2, 1))
    sh = {
        "ident_f": np.eye(128, dtype=np.float32),
        "tri_f": np.triu(np.ones((128, 128), np.float32), 1),
        "blk_start": np.ascontiguousarray(np.broadcast_to((np.arange(NBIG, dtype=np.float32) * BS)[None, :], (128, NBIG))),
        "iota_p": np.arange(128, dtype=np.float32).reshape(128, 1),
        "ada_w": f(inputs["ada_w"]), "ada_b": f(inputs["ada_b"]),
        "a_w_in": f(inputs["a_w_in"]), "a_b_in_c": col(f(inputs["a_b_in"])),
        "a_w_dw_c": np.ascontiguousarray(f(inputs["a_w_dw"]).reshape(2, 31, KC, 128).transpose(0, 3, 2, 1)),
        "a_b_dw_c": col(f(inputs["a_b_dw"])), "a_ln_g_c": col(f(inputs["a_ln_g"])), "a_ln_b_c": col(f(inputs["a_ln_b"])),
        "a_w_out": f(inputs["a_w_out"]), "a_b_out": f(inputs["a_b_out"]),
        "b_w_in": f(inputs["b_w_in"]),
        "b_w_dw_c": np.ascontiguousarray(f(inputs["b_w_dw"]).reshape(2, 3, KC, 128).transpose(0, 3, 2, 1)),
        "b_w_out": f(inputs["b_w_out"]),
        "mix_ln_g": f(inputs["mix_ln_g"]), "mix_ln_b": f(inputs["mix_ln_b"]),
        "ffn_ln_g": f(inputs["ffn_ln_g"]), "ffn_ln_b": f(inputs["ffn_ln_b"]),
        "mix_ln_g_c": col(f(inputs["mix_ln_g"])), "mix_ln_b_c": col(f(inputs["mix_ln_b"])),
        "r_w": np.ascontiguousarray(np.concatenate([f(inputs["r_w_group"]), f(inputs["r_w_expert"])], axis=2)),
        "r_b": np.ascontiguousarray(np.concatenate([f(inputs["r_b_group"]), f(inputs["r_b_expert"])], axis=1)),
    }
    for nm, kk in (("e_w_gate", KC), ("e_w_up", KC), ("e_w_down", 4)):
        a = f(inputs[nm])
        a = a.reshape(DEPTH, NEXP, kk, 128, a.shape[-1]).transpose(0, 1, 3, 2, 4).reshape(DEPTH, NEXP * 128, 4096)
        for l_ in range(DEPTH):
            for hh in range(2):
                sh["%s_%d_%d" % (nm, l_, hh)] = np.ascontiguousarray(a[l_, :, hh * 2048:(hh + 1) * 2048])
    return sh


_PROG_CACHE = {}


def _get_prog(layers, first, last, debug=()):
    key = (tuple(layers), first, last, tuple(debug))
    if key not in _PROG_CACHE:
        _PROG_CACHE[key] = Prog(layers, first, last, debug)
    return _PROG_CACHE[key]


def kernel(**inputs):
    shared = _shared_inputs(inputs)
    prog = _get_prog([0, 1, 2, 3], True, True)
    in_maps = []
    for c in range(NCORES):
        m = dict(shared)
        m.update(_core_inputs(inputs, c))
        in_maps.append(m)
    res = run_bass_kernel_spmd(prog.nc, in_maps, core_ids=list(range(NCORES)))
    out = np.zeros((4, 2 * NOWN, D), np.float32)
    for c in range(NCORES):
        b, half = c // 2, c % 2
        out[b, half * NOWN:(half + 1) * NOWN] = res.results[c]["out"]
    return out
```
